# Optimizing a Trainium2 kernel written in Bass

```python
import jax, jax.numpy as jnp
from jax import lax
import numpy as np

D_MODEL = 1024
BATCH = 8
SEQ = 8192
DEPTH = 2

PLE_DIM = 256
A_HEADS = 8
A_HEAD_DIM = 64
A_WIDTH = A_HEADS * A_HEAD_DIM
A_KV_DIM = 64
IDX_HEADS = 8
IDX_DIM = 64
TOPK_MAX = 256
Q_BLOCK = 128
B_HEADS = 4
B_KEY_DIM = 128
B_VAL_DIM = 256
B_KEY_WIDTH = B_HEADS * B_KEY_DIM
B_VAL_WIDTH = B_HEADS * B_VAL_DIM
GATE_RANK = 16
GATE_TAU = 16.0
CHUNK = 64
EPS = 1e-6

IN_SPLITS = (A_WIDTH, A_KV_DIM, A_KV_DIM, IDX_HEADS * IDX_DIM, IDX_DIM, IDX_HEADS, A_WIDTH,
             B_KEY_WIDTH, B_KEY_WIDTH, B_VAL_WIDTH, GATE_RANK, B_VAL_WIDTH,
             D_MODEL, D_MODEL)
IN_WIDTH = sum(IN_SPLITS)

kernel_name = "hybrid_dsa_gla_gated_merge"

F32 = jnp.float32


def rms_norm(x, g):
    x32 = x.astype(F32)
    y = x32 * lax.rsqrt(jnp.mean(x32 * x32, axis=-1, keepdims=True) + EPS)
    return (y * g.astype(F32)).astype(x.dtype)


def dsa_attention(q, k, v, q_idx, k_idx, w_idx):
    B, L = q.shape[0], q.shape[1]
    top_k = min(TOPK_MAX, L // 4)
    nb = L // Q_BLOCK
    kv = jnp.concatenate([k, v], axis=-1)
    k_idx32 = k_idx.astype(F32)
    key_pos = jnp.arange(L)
    idx_scale = (IDX_HEADS ** -0.5) * (IDX_DIM ** -0.5)
    attn_scale = A_HEAD_DIM ** -0.5

    def to_blocks(t):
        return jnp.moveaxis(t.reshape((B, nb, Q_BLOCK) + t.shape[2:]), 1, 0)

    def block(args):
        qb, qib, wb, start = args
        q_pos = start + jnp.arange(Q_BLOCK)
        causal = key_pos[None, :] <= q_pos[:, None]
        logits = jnp.einsum('bqhd,bsd->bqhs', qib.astype(F32), k_idx32)
        score = jnp.einsum('bqh,bqhs->bqs', wb.astype(F32) * idx_scale, jax.nn.relu(logits))
        score = jnp.where(causal[None], score, -jnp.inf)
        _, idx = lax.top_k(score, top_k)
        sel = jax.vmap(lambda t, i: t[i])(kv, idx)
        k_sel, v_sel = jnp.split(sel, 2, axis=-1)
        valid = idx <= q_pos[None, :, None]
        s = jnp.einsum('bqhd,bqkd->bqhk', qb, k_sel).astype(F32) * attn_scale
        s = jnp.where(valid[:, :, None, :], s, -jnp.inf)
        prob = jax.nn.softmax(s, axis=-1).astype(v_sel.dtype)
        return jnp.einsum('bqhk,bqkd->bqhd', prob, v_sel)

    starts = jnp.arange(nb) * Q_BLOCK
    out = lax.map(block, (to_blocks(q), to_blocks(q_idx), to_blocks(w_idx), starts))
    return jnp.moveaxis(out, 0, 1).reshape(B, L, A_HEADS * A_KV_DIM)


def gla_chunked(q, k, v, log_a):
    B, L, H, dk = q.shape
    dv = v.shape[-1]
    n = L // CHUNK

    def chunks(t):
        return t.astype(F32).reshape(B, n, CHUNK, H, t.shape[-1]).transpose(1, 0, 3, 2, 4)

    causal = jnp.tril(jnp.ones((CHUNK, CHUNK), dtype=bool))

    def step(S, inp):
        qc, kc, vc, ac = inp
        G = jnp.cumsum(ac, axis=2)
        diff = G[:, :, :, None, :] - G[:, :, None, :, :]
        decay = jnp.exp(jnp.where(causal[None, None, :, :, None], diff, -jnp.inf))
        A = jnp.sum(qc[:, :, :, None, :] * kc[:, :, None, :, :] * decay, axis=-1)
        o = (jnp.einsum('bhij,bhje->bhie', A, vc)
             + jnp.einsum('bhid,bhde->bhie', qc * jnp.exp(G), S))
        G_last = G[:, :, -1:, :]
        S = (jnp.exp(G_last[:, :, 0, :])[..., None] * S
             + jnp.einsum('bhcd,bhce->bhde', kc * jnp.exp(G_last - G), vc))
        return S, o

    S0 = jnp.zeros((B, H, dk, dv), F32)
    _, o = lax.scan(step, S0, (chunks(q), chunks(k), chunks(v), chunks(log_a)))
    return o.transpose(1, 0, 3, 2, 4).reshape(B, L, H, dv)


def hybrid_layer(x, p_i, g_pre, w_in, w_gate_up, b_gate, g_gla_head, w_proj_a, w_proj_b,
                 w_out, g_post, w_ple, w_ple_gate, g_ple_pre, g_ple_post):
    B, L, _ = x.shape
    h = rms_norm(x, g_pre)
    z = h @ w_in
    (qa, ka, va, qi, ki, wi, ga, qb, kb, vb, gdown, gb, ma, mb) = jnp.split(
        z, np.cumsum(IN_SPLITS)[:-1].tolist(), axis=-1)

    oa = dsa_attention(qa.reshape(B, L, A_HEADS, A_HEAD_DIM), ka, va,
                       qi.reshape(B, L, IDX_HEADS, IDX_DIM), ki, wi)
    oa = oa.astype(x.dtype) * jax.nn.silu(ga)

    log_a = jax.nn.log_sigmoid((gdown @ w_gate_up + b_gate).astype(F32)) / GATE_TAU
    ob = gla_chunked(qb.reshape(B, L, B_HEADS, B_KEY_DIM) * (B_KEY_DIM ** -0.5),
                     kb.reshape(B, L, B_HEADS, B_KEY_DIM),
                     vb.reshape(B, L, B_HEADS, B_VAL_DIM),
                     log_a.reshape(B, L, B_HEADS, B_KEY_DIM))
    ob = rms_norm(ob.astype(x.dtype), g_gla_head).reshape(B, L, B_VAL_WIDTH) * jax.nn.silu(gb)

    y = jax.nn.sigmoid(ma) * (oa @ w_proj_a) + jax.nn.sigmoid(mb) * (ob @ w_proj_b)
    x = x + rms_norm(y @ w_out, g_post)

    e = (p_i @ w_ple) * jax.nn.sigmoid(rms_norm(x, g_ple_pre) @ w_ple_gate)
    return x + rms_norm(e, g_ple_post)


def setup_inputs(seed: int = 0) -> dict:
    key = jax.random.key(seed)
    ks = jax.random.split(key, 16)

    def w(k, shape, fan_in):
        return jax.random.normal(k, shape, F32) * (fan_in ** -0.5)

    def gain(k, shape):
        return 1.0 + 0.05 * jax.random.normal(k, shape, F32)

    return {
        "x": jax.random.normal(ks[0], (BATCH, SEQ, D_MODEL), F32),
        "p": jax.random.normal(ks[1], (DEPTH, BATCH, SEQ, PLE_DIM), F32),
        "g_pre": gain(ks[2], (DEPTH, D_MODEL)),
        "w_in": w(ks[3], (DEPTH, D_MODEL, IN_WIDTH), D_MODEL),
        "w_gate_up": w(ks[4], (DEPTH, GATE_RANK, B_KEY_WIDTH), GATE_RANK),
        "b_gate": 0.1 * jax.random.normal(ks[5], (DEPTH, B_KEY_WIDTH), F32),
        "g_gla_head": gain(ks[6], (DEPTH, B_VAL_DIM)),
        "w_proj_a": w(ks[7], (DEPTH, A_WIDTH, D_MODEL), A_WIDTH),
        "w_proj_b": w(ks[8], (DEPTH, B_VAL_WIDTH, D_MODEL), B_VAL_WIDTH),
        "w_out": w(ks[9], (DEPTH, D_MODEL, D_MODEL), D_MODEL),
        "g_post": gain(ks[10], (DEPTH, D_MODEL)),
        "w_ple": w(ks[11], (DEPTH, PLE_DIM, D_MODEL), PLE_DIM),
        "w_ple_gate": w(ks[12], (DEPTH, D_MODEL, D_MODEL), D_MODEL),
        "g_ple_pre": gain(ks[13], (DEPTH, D_MODEL)),
        "g_ple_post": gain(ks[14], (DEPTH, D_MODEL)),
    }


def reference(x, p, g_pre, w_in, w_gate_up, b_gate, g_gla_head, w_proj_a, w_proj_b,
              w_out, g_post, w_ple, w_ple_gate, g_ple_pre, g_ple_post):
    for i in range(DEPTH):
        x = hybrid_layer(x, p[i], g_pre[i], w_in[i], w_gate_up[i], b_gate[i], g_gla_head[i],
                         w_proj_a[i], w_proj_b[i], w_out[i], g_post[i], w_ple[i],
                         w_ple_gate[i], g_ple_pre[i], g_ple_post[i])
    return x
```

```python
from contextlib import ExitStack
import math
import types

import numpy as np
import concourse.bass as bass
import concourse.mybir as mybir
from concourse.bass_utils import run_bass_kernel_spmd

F32 = mybir.dt.float32
BF16 = mybir.dt.bfloat16
ALU = mybir.AluOpType
AF = mybir.ActivationFunctionType
AX = mybir.AxisListType

D = 1024
DEPTH = 2
INW = 6872
EPS = 1e-6
NEG = -1.0e30
ENGS = ("pe", "act", "dve", "pool", "sp")

C_QA, C_KA, C_VA, C_QI, C_KI, C_WI, C_GA = 0, 512, 576, 640, 1152, 1216, 1224
C_QB, C_KB, C_VB, C_GD, C_GB, C_MA, C_MB = 1736, 2248, 2760, 3784, 3800, 4824, 5848


class Buf:
    __slots__ = ("name", "w", "r", "dsem", "dcnt", "excl")

    def __init__(self, name, excl=False):
        self.name = name
        self.excl = excl
        self.w = None
        self.r = []
        self.dsem = None
        self.dcnt = 0


def _freeze(fn):
    if fn.__closure__ is None:
        return fn
    cells = []
    for c in fn.__closure__:
        try:
            cells.append(types.CellType(c.cell_contents))
        except ValueError:
            cells.append(c)
    g = types.FunctionType(fn.__code__, fn.__globals__, fn.__name__, fn.__defaults__, tuple(cells))
    g.__kwdefaults__ = fn.__kwdefaults__
    return g


class Prog:
    def __init__(self, nc):
        self.nc = nc
        self.es = ExitStack()
        self.ops = {e: [] for e in ENGS}
        self.sem = {}
        self.cnt = {}
        self.waited = {e: {} for e in ENGS}
        self.semobj = {}
        self.dma_final = {}
        self.dpool = {}
        self.dval = {}
        self.dbufs = []
        self.nsem = 0
        self.phase_id = 0
        self.total = 0
        import os as _os
        self.maxops = int(_os.environ.get("PROG_MAXOPS", 10 ** 9))
        self.nrec = 0
        self.last_desc = None
        self._new_engine_sems()

    def _alloc_sem(self, name):
        s = self.es.enter_context(self.nc.semaphore(name))
        self.semobj[name] = s
        self.nsem += 1
        return name

    def _new_engine_sems(self):
        for e in ENGS:
            self.sem[e] = self._alloc_sem(f"p{self.phase_id}_{e}")
            self.cnt[e] = 0

    def _wait(self, eng, tok):
        if tok is None:
            return
        key, val = tok
        if eng == "pe" and key == self.sem["pe"]:
            return
        w = self.waited[eng]
        if w.get(key, 0) >= val:
            return
        w[key] = val
        self.ops[eng].append(("wait", key, val))

    def _deps(self, eng, reads, writes):
        for b in reads:
            self._wait(eng, b.w)
            if b.excl:
                for t in b.r:
                    self._wait(eng, t)
        for b in writes:
            self._wait(eng, b.w)
            for t in b.r:
                self._wait(eng, t)

    def _mark(self, tok, reads, writes):
        for b in reads:
            b.r.append(tok)
            if len(b.r) > 24:
                b.r = b.r[-24:] if False else b.r
        for b in writes:
            b.w = tok
            b.r = []

    def op(self, eng, fn, reads=(), writes=()):
        if self.nrec >= self.maxops:
            return None
        self.nrec += 1
        self.last_desc = (eng, fn.__code__.co_firstlineno, [b.name for b in reads], [b.name for b in writes])
        self._deps(eng, reads, writes)
        self.cnt[eng] += 1
        tok = (self.sem[eng], self.cnt[eng])
        self.ops[eng].append(("op", _freeze(fn), self.sem[eng], 1, self.cnt[eng]))
        self._mark(tok, reads, writes)
        return tok

    def dma(self, eng, fn, sb, reads=(), writes=()):
        if self.nrec >= self.maxops:
            return None
        self.nrec += 1
        self.last_desc = ("dma-" + eng, fn.__code__.co_firstlineno, [b.name for b in reads], [b.name for b in writes])
        self._deps(eng, reads, writes)
        if sb.dsem is None:
            sb.dsem = {}
        if eng not in sb.dsem:
            fl = self.dpool.setdefault(eng, [])
            if fl:
                sb.dsem[eng] = fl.pop()
            else:
                sb.dsem[eng] = self._alloc_sem(f"d{eng}{self.nsem}")
                self.dval[sb.dsem[eng]] = 0
            self.dbufs.append((sb, eng))
        key = sb.dsem[eng]
        self.dval[key] += 16
        tok = (key, self.dval[key])
        self.dma_final[key] = self.dval[key]
        self.ops[eng].append(("op", _freeze(fn), key, 16, 0))
        self._mark(tok, reads, writes)
        return tok

    def barrier(self):
        toks = [(self.sem[e], self.cnt[e]) for e in ENGS if self.cnt[e] > 0]
        toks += list(self.dma_final.items())
        for e in ENGS:
            for key, val in toks:
                if key == self.sem[e]:
                    continue
                w = self.waited[e]
                if w.get(key, 0) >= val:
                    continue
                w[key] = val
                self.ops[e].append(("wait", key, val))

    def emit(self):
        nc = self.nc
        ops = self.ops
        semobj = self.semobj

        engkeys = {self.sem[e]: e for e in ENGS}
        waited_vals = {k: set() for k in engkeys}
        for e in ENGS:
            for o in ops[e]:
                if o[0] == "wait" and o[1] in engkeys:
                    waited_vals[o[1]].add(o[2])
        if not hasattr(self, "newcnt"):
            self.newcnt = {k: 0 for k in engkeys}
            self.vmap = {}
        for k, e in engkeys.items():
            for o in ops[e]:
                if o[0] == "op" and o[2] == k and o[4] in waited_vals[k]:
                    self.newcnt[k] += 1
                    self.vmap[(k, o[4])] = self.newcnt[k]
        vmap = self.vmap

        def replay(handle, lst):
            for o in lst:
                if o[0] == "wait":
                    if o[1] in engkeys:
                        handle.wait_ge(semobj[o[1]], vmap[(o[1], o[2])])
                    else:
                        handle.wait_ge(semobj[o[1]], o[2])
                else:
                    ins = o[1](handle)
                    if o[2] in engkeys:
                        if (o[2], o[4]) in vmap:
                            ins.then_inc(semobj[o[2]], 1)
                    else:
                        ins.then_inc(semobj[o[2]], o[3])

        with nc.Block() as blk:
            if ops["pe"]:
                blk.tensor(lambda e: replay(e, ops["pe"]))
            if ops["act"]:
                blk.scalar(lambda e: replay(e, ops["act"]))
            if ops["dve"]:
                blk.vector(lambda e: replay(e, ops["dve"]))
            if ops["pool"]:
                blk.gpsimd(lambda e: replay(e, ops["pool"]))
            if ops["sp"]:
                blk.sync(lambda e: replay(e, ops["sp"]))
        n = sum(len(v) for v in ops.values())
        self.total += n
        self.ops = {e: [] for e in ENGS}
        return n

    def end_phase(self):
        self.barrier()
        n = self.emit()
        self.phase_id += 1
        for b, eng in self.dbufs:
            self.dpool[eng].append(b.dsem.pop(eng))
        self.dbufs = []
        return n

    def close(self):
        self.es.close()


class T:
    def __init__(self, t, name):
        self.t = t
        self.b = Buf(name)


def build(L, nlayers=DEPTH, dbg=False, stop_after=None):
    NB = L // 128
    NSB = L // 512
    TOPK = min(256, L // 4)
    NIT = 16
    nc = bass.Bass("TRN2", target_bir_lowering=False)
    P = Prog(nc)

    def dram(name, shape, dt, kind):
        return nc.dram_tensor(name, shape, dt, kind=kind).ap()

    SCR = "ExternalOutput" if dbg else "Internal"
    x_in = dram("x", [L, D], F32, "ExternalInput")
    p_in = dram("p", [DEPTH, L, 256], F32, "ExternalInput")
    w_in = dram("w_in", [DEPTH, D, INW], F32, "ExternalInput")
    gpreT = dram("gpreT", [DEPTH, 128, 8], F32, "ExternalInput")
    wgu = dram("w_gate_up", [DEPTH, 16, 512], F32, "ExternalInput")
    bgate = dram("b_gate", [DEPTH, 512], F32, "ExternalInput")
    gglaT = dram("gglaT", [DEPTH, 128, 2], F32, "ExternalInput")
    w_pa = dram("w_proj_a", [DEPTH, 512, D], F32, "ExternalInput")
    w_pb = dram("w_proj_b", [DEPTH, D, D], F32, "ExternalInput")
    w_o = dram("w_out", [DEPTH, D, D], F32, "ExternalInput")
    g_post = dram("g_post", [DEPTH, D], F32, "ExternalInput")
    w_ple = dram("w_ple", [DEPTH, 256, D], F32, "ExternalInput")
    w_pg = dram("w_ple_gate", [DEPTH, D, D], F32, "ExternalInput")
    gppT = dram("gppT", [DEPTH, 128, 8], F32, "ExternalInput")
    g_pp = dram("g_ple_post", [DEPTH, D], F32, "ExternalInput")
    c_ident = dram("c_ident", [128, 128], F32, "ExternalInput")
    c_tri = dram("c_tri", [128, 128], F32, "ExternalInput")
    c_triu = dram("c_triu", [128, 128], F32, "ExternalInput")
    c_cmask = dram("c_cmask", [128, 128], F32, "ExternalInput")
    c_pw = dram("c_pw", [128, NIT], F32, "ExternalInput")
    y_out = dram("y", [L, D], F32, "ExternalOutput")

    s_qiT = dram("s_qiT", [NB, 128, 4, 128], BF16, SCR)
    s_qaT = dram("s_qaT", [NB, 128, 4, 128], BF16, SCR)
    s_qgT = dram("s_qgT", [NB, 128, 4, 128], BF16, SCR)
    s_kgT = dram("s_kgT", [NB, 128, 4, 128], BF16, SCR)
    s_kl = dram("s_kl", [NB, 128, 512], BF16, SCR)
    s_vb = dram("s_vb", [NB, 128, 1024], BF16, SCR)
    s_sga = dram("s_sga", [NB, 128, 512], F32, SCR)
    s_sgb = dram("s_sgb", [NB, 128, 1024], F32, SCR)
    s_sma = dram("s_sma", [NB, 128, 1024], F32, SCR)
    s_smb = dram("s_smb", [NB, 128, 1024], F32, SCR)
    s_oagT = dram("s_oagT", [NB, 128, 4, 128], BF16, SCR)
    s_x1 = dram("s_x1", [L, D], F32, SCR)

    glob = ExitStack()

    def sbuf(es, name, shape, dt):
        return T(es.enter_context(nc.sbuf_tensor(name, shape, dt)), name)

    identf = sbuf(glob, "identf", [128, 128], F32)
    identb = sbuf(glob, "identb", [128, 128], BF16)
    trif = sbuf(glob, "trif", [128, 128], F32)
    triuf = sbuf(glob, "triuf", [128, 128], F32)
    cmask = sbuf(glob, "cmask", [128, 128], F32)
    pw = sbuf(glob, "pw", [128, NIT], F32)
    neghalf = sbuf(glob, "neghalf", [128, 8], F32)
    onesrow = sbuf(glob, "onesrow", [1, 128], F32)
    eGl = sbuf(glob, "eGl", [128, NB, 4], F32)

    ld = lambda dst_T, dst_ap, src_ap: P.dma("sp", lambda e: e.dma_start(out=dst_ap, in_=src_ap), dst_T.b, writes=[dst_T.b])

    ld(identf, identf.t[:], c_ident)
    ld(trif, trif.t[:], c_tri)
    ld(triuf, triuf.t[:], c_triu)
    ld(cmask, cmask.t[:], c_cmask)
    ld(pw, pw.t[:], c_pw)
    P.op("dve", lambda e: e.tensor_copy(out=identb.t[:], in_=identf.t[:]), reads=[identf.b], writes=[identb.b])
    P.op("dve", lambda e: e.memset(neghalf.t[:], -0.5), writes=[neghalf.b])
    P.op("dve", lambda e: e.memset(onesrow.t[:], 1.0), writes=[onesrow.b])

    def rsqrt_col(src_T, src_ap, dst_T, dst_ap, n, inv_n, ncols=1):
        P.op("dve", lambda e: e.tensor_scalar(out=dst_ap, in0=src_ap, scalar1=inv_n, scalar2=EPS, op0=ALU.mult, op1=ALU.add),
             reads=[src_T.b], writes=[dst_T.b])
        P.op("pool", lambda e: e.tensor_tensor(out=dst_ap, in0=dst_ap, in1=neghalf.t[:, 0:ncols], op=ALU.pow),
             reads=[dst_T.b, neghalf.b], writes=[dst_T.b])

    for l in range(nlayers):
        x_src = x_in if l == 0 else s_x1
        x_dst = s_x1 if l < nlayers - 1 else y_out
        if nlayers == 1:
            x_dst = y_out

        resid = ExitStack()
        kiT = sbuf(resid, f"kiT{l}", [128, L], BF16)
        kaT = sbuf(resid, f"kaT{l}", [128, L], BF16)
        va = sbuf(resid, f"va{l}", [128, NB, 65], BF16)
        wi = sbuf(resid, f"wi{l}", [128, NB, 8], F32)

        import os as _os
        for cpass in [int(c) for c in _os.environ.get('A_PASSES', '01')]:
            pa = ExitStack()
            if cpass == 0:
                srcs = [(C_QA, 512), (C_QI, 512), (C_KA, 64), (C_KA, 64), (C_KI, 64), (C_KI, 64),
                        (C_QB, 512), (C_KB, 512), (C_GD, 16), (C_VA, 64), (C_WI, 8), (C_VB, 1024)]
            else:
                srcs = [(C_GA, 512), (C_GB, 1024), (C_MA, 1024), (C_MB, 1024)]
            offs = []
            o = 0
            for (c0, w) in srcs:
                offs.append(o)
                o += w
            WCOLS = o
            Wb = sbuf(pa, f"Wb{l}_{cpass}", [128, 8, WCOLS], BF16)
            wst = [sbuf(pa, f"wst{l}_{cpass}_{i}", [128, 1024], F32) for i in range(2)]
            gcol = sbuf(pa, f"gcol{l}_{cpass}", [128, 8], F32)
            ps = T(pa.enter_context(nc.psum_tensor(f"psA{l}_{cpass}", [128, 8, 512], F32)), "psA")
            pb = [Buf(f"pb{i}", excl=True) for i in range(8)]
            ld(gcol, gcol.t[:], gpreT[l])
            ci = 0
            for k in range(8):
                for (c0, w), o in zip(srcs, offs):
                    st = wst[ci % 2]
                    P.dma("sp", lambda e, st=st, k=k, c0=c0, w=w: e.dma_start(out=st.t[:, 0:w], in_=w_in[l, k * 128:(k + 1) * 128, c0:c0 + w]),
                          st.b, writes=[st.b])
                    if ci % 2 == 0:
                        P.op("dve", lambda e, st=st, k=k, o=o, w=w: e.tensor_scalar(out=Wb.t[:, k, o:o + w], in0=st.t[:, 0:w], scalar1=gcol.t[:, k:k + 1], scalar2=None, op0=ALU.mult),
                             reads=[st.b, gcol.b], writes=[Wb.b])
                    else:
                        P.op("act", lambda e, st=st, k=k, o=o, w=w: e.activation(out=Wb.t[:, k, o:o + w], in_=st.t[:, 0:w], func=AF.Copy, scale=gcol.t[:, k:k + 1]),
                             reads=[st.b, gcol.b], writes=[Wb.b])
                    ci += 1

            xs = [sbuf(pa, f"xs{l}_{cpass}_{i}", [128, D], F32) for i in range(2)]
            junk = sbuf(pa, f"junk{l}_{cpass}", [128, D], BF16)
            ssq = sbuf(pa, f"ssq{l}_{cpass}", [128, 1], F32)
            rstd = sbuf(pa, f"rstd{l}_{cpass}", [128, 1], F32)
            hn = sbuf(pa, f"hn{l}_{cpass}", [128, D], BF16)
            hT = [sbuf(pa, f"hT{l}_{cpass}_{i}", [128, 8, 512], BF16) for i in range(2)]
            psT = ps.t[:, 0, :].bitcast(BF16)
            rot = [1, 2, 3, 4]
            rc = [0]

            def nextbank():
                b = rot[rc[0] % 4]
                rc[0] += 1
                return b

            if cpass == 0:
                fm_st = {n: sbuf(pa, f"fm_{n}{l}", [128, 4, 4, 128], BF16) for n in ("qa", "qi", "qg", "kg")}
                gdT = sbuf(pa, f"gdT{l}", [16, 512], F32)
                wgu_s = sbuf(pa, f"wgu{l}", [16, 512], F32)
                bg_s = sbuf(pa, f"bg{l}", [1, 512], F32)
                e1 = sbuf(pa, f"e1_{l}", [128, 512], F32)
                sp_ = sbuf(pa, f"sp_{l}", [128, 512], F32)
                E1 = sbuf(pa, f"E1_{l}", [128, 4, 128], F32)
                E2 = sbuf(pa, f"E2_{l}", [128, 4, 128], F32)
                E3 = sbuf(pa, f"E3_{l}", [128, 512], F32)
                kl_st = [sbuf(pa, f"klst{l}_{i}", [128, 512], BF16) for i in range(2)]
                vb_st = [sbuf(pa, f"vbst{l}_{i}", [128, 1024], BF16) for i in range(2)]
                ld(wgu_s, wgu_s.t[:], wgu[l])
                ld(bg_s, bg_s.t[:], bgate[l:l + 1, :])
                P.op("pool", lambda e: e.memset(va.t[:, :, 64:65], 1.0), writes=[va.b])
                O_QA, O_QI, O_KA, O_KI, O_QB, O_KB, O_GD, O_VA, O_WI, O_VB = (
                    offs[0], offs[1], offs[2], offs[4], offs[6], offs[7], offs[8], offs[9], offs[10], offs[11])
            else:
                g_st = {n: [sbuf(pa, f"gst_{n}{l}_{i}", [128, w], F32) for i in range(2)]
                        for n, w in (("ga", 512), ("gb", 1024), ("ma", 1024), ("mb", 1024))}
                O_GA, O_GB, O_MA, O_MB = offs

            for sb in range(int(_os.environ.get('A_NSB', NSB))):
                h_T = hT[sb % 2]
                for tb in range(4):
                    blk = sb * 4 + tb
                    xb = xs[blk % 2]
                    ld(xb, xb.t[:], x_src[blk * 128:(blk + 1) * 128, :])
                    P.op("act", lambda e, xb=xb: e.activation(out=junk.t[:], in_=xb.t[:], func=AF.Square, accum_out=ssq.t[:]),
                         reads=[xb.b], writes=[junk.b, ssq.b])
                    rsqrt_col(ssq, ssq.t[:], rstd, rstd.t[:], 1, 1.0 / D)
                    P.op("dve", lambda e, xb=xb: e.tensor_scalar(out=hn.t[:], in0=xb.t[:], scalar1=rstd.t[:], scalar2=None, op0=ALU.mult),
                         reads=[xb.b, rstd.b], writes=[hn.b])
                    for k in range(8):
                        P.op("pe", lambda e, k=k: e.transpose(out=psT[:, k * 128:(k + 1) * 128], in_=hn.t[:, k * 128:(k + 1) * 128], identity=identb.t[:]),
                             reads=[hn.b, identb.b], writes=[pb[0]])
                    P.op("act", lambda e, tb=tb, h_T=h_T: e.copy(out=h_T.t[:, :, tb * 128:(tb + 1) * 128], in_=psT.rearrange("p (k t) -> p k t", k=8)),
                         reads=[pb[0]], writes=[h_T.b])

                def proj_fm(o, m, bank, ncols=512, t0=0):
                    for k in range(8):
                        P.op("pe", lambda e, k=k: e.matmul(out=ps.t[0:m, bank, 0:ncols], lhsT=Wb.t[:, k, o:o + m], rhs=h_T.t[:, k, t0:t0 + ncols], start=(k == 0), stop=(k == 7)),
                             reads=[Wb.b, h_T.b], writes=[pb[bank]])

                def proj_tm(o, n, tb, bank):
                    for k in range(8):
                        P.op("pe", lambda e, k=k: e.matmul(out=ps.t[:, bank, 0:n], lhsT=h_T.t[:, k, tb * 128:(tb + 1) * 128], rhs=Wb.t[:, k, o:o + n], start=(k == 0), stop=(k == 7)),
                             reads=[Wb.b, h_T.b], writes=[pb[bank]])

                if cpass == 0:
                    for name, o0 in (("qa", O_QA), ("qi", O_QI)):
                        st = fm_st[name]
                        for c in range(4):
                            bank = nextbank()
                            proj_fm(o0 + c * 128, 128, bank)
                            eng = "act" if c % 2 == 0 else "dve"
                            if eng == "act":
                                P.op("act", lambda e, st=st, c=c, bank=bank: e.copy(out=st.t[:, :, c, :], in_=ps.t[:, bank, :].rearrange("p (a t) -> p a t", a=4)),
                                     reads=[pb[bank]], writes=[st.b])
                            else:
                                P.op("dve", lambda e, st=st, c=c, bank=bank: e.tensor_copy(out=st.t[:, :, c, :], in_=ps.t[:, bank, :].rearrange("p (a t) -> p a t", a=4)),
                                     reads=[pb[bank]], writes=[st.b])
                    for res, o0 in ((kaT, O_KA), (kiT, O_KI)):
                        bank = nextbank()
                        proj_fm(o0, 128, bank)
                        _v = _os.environ.get("V_KA", "")
                        _sbw = 0 if _v == "same" else sb
                        if _v == "dve":
                            P.op("dve", lambda e, res=res, bank=bank: e.tensor_copy(out=res.t[:, sb * 512:(sb + 1) * 512], in_=ps.t[:, bank, :]),
                                 reads=[pb[bank]], writes=[res.b])
                        elif _v == "nodep":
                            P.op("act", lambda e, res=res, bank=bank: e.copy(out=res.t[:, sb * 512:(sb + 1) * 512], in_=ps.t[:, bank, :]),
                                 reads=[pb[bank]], writes=[])
                        else:
                            P.op("act", lambda e, res=res, bank=bank, _sbw=_sbw: e.copy(out=res.t[:, _sbw * 512:(_sbw + 1) * 512], in_=ps.t[:, bank, :]),
                                 reads=[pb[bank]], writes=[res.b])
                    bank = nextbank()
                    proj_fm(O_GD, 16, bank)
                    P.op("dve", lambda e, bank=bank: e.tensor_copy(out=gdT.t[:], in_=ps.t[0:16, bank, :]), reads=[pb[bank]], writes=[gdT.b])

                    for tb in range(4):
                        blk = sb * 4 + tb
                        tsl = slice(tb * 128, (tb + 1) * 128)
                        bank = nextbank()
                        proj_tm(O_VA, 72, tb, bank)
                        P.op("act", lambda e, bank=bank, blk=blk: e.copy(out=va.t[:, blk, 0:64], in_=ps.t[:, bank, 0:64]), reads=[pb[bank]], writes=[va.b])
                        P.op("dve", lambda e, bank=bank, blk=blk: e.tensor_scalar(out=wi.t[:, blk, :], in0=ps.t[:, bank, 64:72], scalar1=float(8 ** -0.5 * 64 ** -0.5), scalar2=None, op0=ALU.mult),
                             reads=[pb[bank]], writes=[wi.b])
                        vst = vb_st[blk % 2]
                        for hf in range(2):
                            bank = nextbank()
                            proj_tm(O_VB + hf * 512, 512, tb, bank)
                            if hf == 0:
                                P.op("act", lambda e, bank=bank, vst=vst: e.copy(out=vst.t[:, 0:512], in_=ps.t[:, bank, :]), reads=[pb[bank]], writes=[vst.b])
                            else:
                                P.op("dve", lambda e, bank=bank, vst=vst: e.tensor_copy(out=vst.t[:, 512:1024], in_=ps.t[:, bank, :]), reads=[pb[bank]], writes=[vst.b])
                        P.dma("pool", lambda e, vst=vst, blk=blk: e.dma_start(out=s_vb[blk], in_=vst.t[:]), vst.b, reads=[vst.b])
                        P.op("pe", lambda e, tsl=tsl: e.matmul(out=ps.t[:, 5, :], lhsT=gdT.t[:, tsl], rhs=wgu_s.t[:], start=True, stop=False),
                             reads=[gdT.b, wgu_s.b], writes=[pb[5]])
                        P.op("pe", lambda e: e.matmul(out=ps.t[:, 5, :], lhsT=onesrow.t[:], rhs=bg_s.t[:], start=False, stop=True),
                             reads=[onesrow.b, bg_s.b], writes=[pb[5]])
                        P.op("act", lambda e: e.activation(out=e1.t[:], in_=ps.t[:, 5, :], func=AF.Exp, scale=-1.0), reads=[pb[5]], writes=[e1.b])
                        P.op("act", lambda e: e.activation(out=sp_.t[:], in_=e1.t[:], func=AF.Ln, bias=1.0), reads=[e1.b], writes=[sp_.b])
                        for h in range(4):
                            P.op("pe", lambda e, h=h: e.matmul(out=ps.t[:, 6, h * 128:(h + 1) * 128], lhsT=sp_.t[:, h * 128:(h + 1) * 128], rhs=trif.t[:], start=True, stop=True),
                                 reads=[sp_.b, trif.b], writes=[pb[6]])
                        P.op("pe", lambda e: e.matmul(out=ps.t[:, 5, :], lhsT=triuf.t[:], rhs=sp_.t[:], start=True, stop=True),
                             reads=[triuf.b, sp_.b], writes=[pb[5]])
                        lnsc = float(math.log(128 ** -0.5))
                        P.op("act", lambda e: e.activation(out=E1.t[:], in_=ps.t[:, 6, :].rearrange("p (h t) -> p h t", h=4), func=AF.Exp, scale=-1.0 / 16, bias=lnsc),
                             reads=[pb[6]], writes=[E1.b])
                        P.op("act", lambda e: e.activation(out=E2.t[:], in_=ps.t[:, 6, :].rearrange("p (h t) -> p h t", h=4), func=AF.Exp, scale=1.0 / 16),
                             reads=[pb[6]], writes=[E2.b])
                        P.op("act", lambda e, blk=blk: e.activation(out=eGl.t[:, blk, :], in_=ps.t[:, 6, :].rearrange("p (h t) -> p h t", h=4)[:, :, 127], func=AF.Exp, scale=-1.0 / 16),
                             reads=[pb[6]], writes=[eGl.b])
                        P.op("act", lambda e: e.activation(out=E3.t[:], in_=ps.t[:, 5, :], func=AF.Exp, scale=-1.0 / 16), reads=[pb[5]], writes=[E3.b])
                        for name, o0, Eg in (("qg", O_QB, E1), ("kg", O_KB, E2)):
                            st = fm_st[name]
                            for h in range(4):
                                for k in range(8):
                                    P.op("pe", lambda e, k=k, h=h, o0=o0: e.matmul(out=ps.t[:, 7, h * 128:(h + 1) * 128], lhsT=Wb.t[:, k, o0 + h * 128:o0 + (h + 1) * 128], rhs=h_T.t[:, k, tsl], start=(k == 0), stop=(k == 7)),
                                         reads=[Wb.b, h_T.b], writes=[pb[7]])
                            P.op("dve", lambda e, st=st, Eg=Eg, tb=tb: e.tensor_tensor(out=st.t[:, tb, :, :], in0=ps.t[:, 7, :].rearrange("p (h t) -> p h t", h=4), in1=Eg.t[:], op=ALU.mult),
                                 reads=[pb[7], Eg.b], writes=[st.b])
                        bank = nextbank()
                        proj_tm(O_KB, 512, tb, bank)
                        kst = kl_st[blk % 2]
                        P.op("dve", lambda e, bank=bank, kst=kst: e.tensor_tensor(out=kst.t[:], in0=ps.t[:, bank, :], in1=E3.t[:], op=ALU.mult),
                             reads=[pb[bank], E3.b], writes=[kst.b])
                        P.dma("pool", lambda e, kst=kst, blk=blk: e.dma_start(out=s_kl[blk], in_=kst.t[:]), kst.b, reads=[kst.b])
                    for name, dst in (("qa", s_qaT), ("qi", s_qiT), ("qg", s_qgT), ("kg", s_kgT)):
                        st = fm_st[name]
                        P.dma("pool", lambda e, st=st, dst=dst: e.dma_start(out=dst[sb * 4:(sb + 1) * 4].rearrange("a p c t -> p a c t"), in_=st.t[:]),
                              st.b, reads=[st.b])
                else:
                    for tb in range(4):
                        blk = sb * 4 + tb
                        for name, o0, w, fn, dst in (("ga", O_GA, 512, AF.Silu, s_sga), ("gb", O_GB, 1024, AF.Silu, s_sgb),
                                                     ("ma", O_MA, 1024, AF.Sigmoid, s_sma), ("mb", O_MB, 1024, AF.Sigmoid, s_smb)):
                            st = g_st[name][blk % 2]
                            for hf in range(w // 512):
                                bank = nextbank()
                                proj_tm(o0 + hf * 512, 512, tb, bank)
                                P.op("act", lambda e, st=st, hf=hf, bank=bank, fn=fn: e.activation(out=st.t[:, hf * 512:(hf + 1) * 512], in_=ps.t[:, bank, :], func=fn),
                                     reads=[pb[bank]], writes=[st.b])
                            P.dma("pool", lambda e, st=st, dst=dst, blk=blk: e.dma_start(out=dst[blk], in_=st.t[:]), st.b, reads=[st.b])
            P.end_phase()
            pa.close()
        if stop_after == "A":
            resid.close()
            break

        pb1 = ExitStack()
        ps = T(pb1.enter_context(nc.psum_tensor(f"psB{l}", [128, 8, 512], F32)), "psB")
        pb = [Buf(f"pbB{i}", excl=True) for i in range(8)]
        psT = ps.t[:, 3, :].bitcast(BF16)
        scoreB = [sbuf(pb1, f"score{l}_{i}", [128, L], F32) for i in range(2)]
        maskq = sbuf(pb1, f"maskq{l}", [128, L], BF16)
        maskT = [sbuf(pb1, f"maskT{l}_{i}", [128, NB, 128], BF16) for i in range(2)]
        Rb = [sbuf(pb1, f"Rb{l}_{i}", [128, 512], BF16) for i in range(2)]
        dg = [sbuf(pb1, f"dg{l}_{i}", [128, 8, 128], BF16) for i in range(2)]
        qiB = [sbuf(pb1, f"qiB{l}_{i}", [128, 4, 128], BF16) for i in range(2)]
        qaB = [sbuf(pb1, f"qaB{l}_{i}", [128, 4, 128], BF16) for i in range(2)]
        sgaB = [sbuf(pb1, f"sgaB{l}_{i}", [128, 512], F32) for i in range(2)]
        qaEB = [sbuf(pb1, f"qaEB{l}_{i}", [128, 4, 128], BF16) for i in range(2)]
        qaOB = [sbuf(pb1, f"qaOB{l}_{i}", [128, 4, 128], BF16) for i in range(2)]
        qiEB = [sbuf(pb1, f"qiEB{l}_{i}", [128, 4, 128], BF16) for i in range(2)]
        qiOB = [sbuf(pb1, f"qiOB{l}_{i}", [128, 4, 128], BF16) for i in range(2)]
        Pt = [sbuf(pb1, f"Pt{l}_{i}", [128, 8, 128], BF16) for i in range(2)]
        lo = sbuf(pb1, f"lo{l}", [128, 1], F32)
        w0 = sbuf(pb1, f"w0{l}", [128, 1], F32)
        WH = sbuf(pb1, f"WH{l}", [128, NIT], F32)
        mid = sbuf(pb1, f"mid{l}", [128, 1], F32)
        cnt = sbuf(pb1, f"cnt{l}", [128, 1], F32)
        stp = sbuf(pb1, f"stp{l}", [128, 1], F32)
        tau = [sbuf(pb1, f"tau{l}_{i}", [128, 1], F32) for i in range(2)]
        rinv = sbuf(pb1, f"rinv{l}", [128, 8], F32)
        oan = sbuf(pb1, f"oan{l}", [128, 8, 64], F32)
        oag = sbuf(pb1, f"oag{l}", [128, 512], BF16)
        oagT = [sbuf(pb1, f"oagT{l}_{i}", [128, 4, 128], BF16) for i in range(2)]
        att_scale = float(64 ** -0.5)

        import os as _os
        PVS = int(_os.environ.get("PVS", 66))

        def idx_stage(qb):
            S = (qb + 1) * 128
            score = scoreB[qb % 2]
            qi_ = qiB[qb % 2]
            dg_ = dg[qb % 2]
            ld(qi_, qi_.t[:], s_qiT[qb])
            qa_ = qaB[qb % 2]
            ld(qa_, qa_.t[:], s_qaT[qb])
            sg_ = sgaB[qb % 2]
            ld(sg_, sg_.t[:], s_sga[qb])
            qaE_ = qaEB[qb % 2]
            qaO_ = qaOB[qb % 2]
            P.op("pool", lambda e: e.tensor_copy(out=qaE_.t[:], in_=qa_.t[:]), reads=[qa_.b], writes=[qaE_.b])
            P.op("pool", lambda e: e.memset(qaE_.t[64:128, :, :], 0.0), writes=[qaE_.b])
            P.op("pool", lambda e: e.tensor_copy(out=qaO_.t[:], in_=qa_.t[:]), reads=[qa_.b], writes=[qaO_.b])
            P.op("pool", lambda e: e.memset(qaO_.t[0:64, :, :], 0.0), writes=[qaO_.b])
            qiE_ = qiEB[qb % 2]
            qiO_ = qiOB[qb % 2]
            P.op("pool", lambda e: e.tensor_copy(out=qiE_.t[:], in_=qi_.t[:]), reads=[qi_.b], writes=[qiE_.b])
            P.op("pool", lambda e: e.memset(qiE_.t[64:128, :, :], 0.0), writes=[qiE_.b])
            P.op("pool", lambda e: e.tensor_copy(out=qiO_.t[:], in_=qi_.t[:]), reads=[qi_.b], writes=[qiO_.b])
            P.op("pool", lambda e: e.memset(qiO_.t[0:64, :, :], 0.0), writes=[qiO_.b])
            P.op("dve", lambda e: e.tensor_tensor(out=dg_.t[:], in0=identb.t[:].unsqueeze(1).to_broadcast([128, 8, 128]),
                                                   in1=wi.t[:, qb, :].unsqueeze(2).to_broadcast([128, 8, 128]), op=ALU.mult),
                 reads=[identb.b, wi.b], writes=[dg_.b])
            nt = (S + 511) // 512
            for kt in range(nt):
                k0 = kt * 512
                wd = min(512, S - k0)
                for h in range(8):
                    hp = (h % 2) * 64
                    bank = h % 2
                    R = Rb[h % 2]
                    P.op("pe", lambda e, h=h, hp=hp, bank=bank: e.matmul(out=ps.t[:, bank, 0:wd], lhsT=(qiE_ if h % 2 == 0 else qiO_).t[:, h // 2, :], rhs=kiT.t[:, k0:k0 + wd], start=True, stop=True),
                         reads=[qiE_.b, qiO_.b, kiT.b], writes=[pb[bank]])
                    P.op("act", lambda e, bank=bank, R=R: e.activation(out=R.t[:, 0:wd], in_=ps.t[:, bank, 0:wd], func=AF.Relu),
                         reads=[pb[bank]], writes=[R.b])
                    P.op("pe", lambda e, h=h, R=R: e.matmul(out=ps.t[:, 2, 0:wd], lhsT=dg_.t[:, h, :], rhs=R.t[:, 0:wd], start=(h == 0), stop=(h == 7)),
                         reads=[dg_.b, R.b], writes=[pb[2]])
                P.op("act", lambda e: e.copy(out=score.t[:, k0:k0 + wd], in_=ps.t[:, 2, 0:wd]), reads=[pb[2]], writes=[score.b])
            P.op("pool", lambda e: e.tensor_tensor(out=score.t[:, qb * 128:S], in0=score.t[:, qb * 128:S], in1=cmask.t[:], op=ALU.add),
                 reads=[score.b, cmask.b], writes=[score.b])

        def select_stage(qb):
            S = (qb + 1) * 128
            score = scoreB[qb % 2]
            ta = tau[qb % 2]
            mT = maskT[qb % 2]
            if S > TOPK:
                SV = qb * 128
                P.op("dve", lambda e: e.tensor_reduce(out=lo.t[:], in_=score.t[:, 0:SV], axis=AX.X, op=ALU.min), reads=[score.b], writes=[lo.b])
                P.op("dve", lambda e: e.tensor_reduce(out=w0.t[:], in_=score.t[:, 0:S], axis=AX.X, op=ALU.max), reads=[score.b], writes=[w0.b])
                P.op("dve", lambda e: e.tensor_tensor(out=w0.t[:], in0=w0.t[:], in1=lo.t[:], op=ALU.subtract), reads=[w0.b, lo.b], writes=[w0.b])
                P.op("dve", lambda e: e.tensor_scalar(out=WH.t[:], in0=pw.t[:], scalar1=w0.t[:], scalar2=None, op0=ALU.mult), reads=[pw.b, w0.b], writes=[WH.b])
                for it in range(NIT):
                    P.op("dve", lambda e, it=it: e.tensor_tensor(out=mid.t[:], in0=lo.t[:], in1=WH.t[:, it:it + 1], op=ALU.add), reads=[lo.b, WH.b], writes=[mid.b])
                    P.op("dve", lambda e: e.tensor_scalar(out=maskq.t[:, 0:S], in0=score.t[:, 0:S], scalar1=mid.t[:], scalar2=None, op0=ALU.is_ge, op1=ALU.add, accum_out=cnt.t[:]),
                         reads=[score.b, mid.b], writes=[maskq.b, cnt.b])
                    P.op("dve", lambda e, it=it: e.tensor_scalar(out=stp.t[:], in0=cnt.t[:], scalar1=float(TOPK) - 0.5, scalar2=WH.t[:, it:it + 1], op0=ALU.is_ge, op1=ALU.mult),
                         reads=[cnt.b, WH.b], writes=[stp.b])
                    P.op("dve", lambda e: e.tensor_tensor(out=lo.t[:], in0=lo.t[:], in1=stp.t[:], op=ALU.add), reads=[lo.b, stp.b], writes=[lo.b])
                    yield
                P.op("dve", lambda e: e.tensor_copy(out=ta.t[:], in_=lo.t[:]), reads=[lo.b], writes=[ta.b])
            else:
                P.op("dve", lambda e: e.memset(ta.t[:], -1.0e29), writes=[ta.b])
            P.op("dve", lambda e: e.tensor_scalar(out=maskq.t[:, 0:S], in0=score.t[:, 0:S], scalar1=ta.t[:], scalar2=None, op0=ALU.is_ge),
                 reads=[score.b, ta.b], writes=[maskq.b])
            for g0 in range(0, qb + 1, 8):
                n = min(8, qb + 1 - g0)
                for j in range(n):
                    kt = g0 + j
                    P.op("pe", lambda e, j=j, kt=kt: e.transpose(out=psT[:, j * 128:(j + 1) * 128], in_=maskq.t[:, kt * 128:(kt + 1) * 128], identity=identb.t[:]),
                         reads=[maskq.b, identb.b], writes=[pb[3]])
                P.op("act", lambda e, g0=g0, n=n: e.copy(out=mT.t[:, g0:g0 + n, :], in_=psT[:, 0:n * 128].rearrange("p (a t) -> p a t", a=n)),
                     reads=[pb[3]], writes=[mT.b])

        def attn_stage(qb):
            qa_ = qaB[qb % 2]
            sg_ = sgaB[qb % 2]
            mT = maskT[qb % 2]
            for kt in range(qb + 1):
                Pm = Pt[kt % 2]
                for h in range(0 if _os.environ.get("V_NOMM") else 8):
                    hp = (h % 2) * 64
                    if _os.environ.get("V_HP0"):
                        hp = 0
                    bank = (0 if _os.environ.get("V_BANK01") else 4) + h // 4
                    P.op("pe", lambda e, h=h, hp=hp, bank=bank: e.matmul(out=ps.t[:, bank, (h % 4) * 128:(h % 4 + 1) * 128], lhsT=kaT.t[:, kt * 128:(kt + 1) * 128],
                                                                       rhs=(qaEB[qb % 2] if h % 2 == 0 else qaOB[qb % 2]).t[:, h // 2, :], start=True, stop=True),
                         reads=[kaT.b, qaEB[qb % 2].b, qaOB[qb % 2].b], writes=[pb[bank]])
                for g in range(0 if _os.environ.get("V_NOEXP") else 2):
                    P.op("act", lambda e, g=g, Pm=Pm: e.activation(out=Pm.t[:, g * 4:(g + 1) * 4, :], in_=ps.t[:, (0 if _os.environ.get("V_BANK01") else 4) + g, :].rearrange("p (h t) -> p h t", h=4), func=(AF.Copy if _os.environ.get("V_COPY") else AF.Exp), scale=att_scale),
                         reads=[pb[(0 if _os.environ.get("V_BANK01") else 4) + g]], writes=[Pm.b])
                _al = int(_os.environ.get("ATT_LEVEL", 3))
                _pvn = int(_os.environ.get("PVN", 65))
                if _al >= 2:
                    P.op("dve", lambda e, Pm=Pm: e.tensor_tensor(out=Pm.t[:], in0=Pm.t[:], in1=mT.t[:, kt, :].unsqueeze(1).to_broadcast([128, 8, 128]), op=ALU.mult),
                         reads=[Pm.b, mT.b], writes=[Pm.b])
                for h in range(8 if _al >= 3 else 0):
                    bank = 6 + h // 4
                    c0 = (h % 4) * PVS
                    P.op("pe", lambda e, h=h, bank=bank, c0=c0, Pm=Pm: e.matmul(out=ps.t[:, bank, c0:c0 + _pvn], lhsT=Pm.t[:, h, :], rhs=va.t[:, kt, 0:_pvn],
                                                                              start=(kt == 0 and h % 4 == 0), stop=(kt == qb), skip_group_check=True),
                         reads=[Pm.b, va.b], writes=[pb[bank]])
                yield
            if _os.environ.get("B1_FIN", "1") == "0":
                return
            pv = ps.t[:, 6:8, 0:4 * PVS].rearrange("p b (h d) -> p b h d", d=PVS)
            P.op("dve", lambda e: e.reciprocal(out=rinv.t[:].rearrange("p (b h o) -> p b h o", b=2, o=1), in_=pv[:, :, :, 64:65]),
                 reads=[pb[6], pb[7]], writes=[rinv.b])
            for b2 in range(2):
                P.op("dve", lambda e, b2=b2: e.tensor_tensor(out=oan.t[:, b2 * 4:(b2 + 1) * 4, :], in0=pv[:, b2, :, 0:64],
                                                           in1=rinv.t[:, b2 * 4:(b2 + 1) * 4].unsqueeze(2).to_broadcast([128, 4, 64]), op=ALU.mult),
                     reads=[pb[6 + b2], rinv.b], writes=[oan.b])
            P.op("dve", lambda e: e.tensor_tensor(out=oag.t[:], in0=oan.t[:].rearrange("p h d -> p (h d)"), in1=sg_.t[:], op=ALU.mult),
                 reads=[oan.b, sg_.b], writes=[oag.b])
            oT = oagT[qb % 2]
            for c in range(4):
                P.op("pe", lambda e, c=c: e.transpose(out=psT[:, c * 128:(c + 1) * 128], in_=oag.t[:, c * 128:(c + 1) * 128], identity=identb.t[:]),
                     reads=[oag.b, identb.b], writes=[pb[3]])
            P.op("act", lambda e: e.copy(out=oT.t[:], in_=psT[:, 0:512].rearrange("p (c t) -> p c t", c=4)), reads=[pb[3]], writes=[oT.b])
            P.dma("pool", lambda e: e.dma_start(out=s_oagT[qb], in_=oT.t[:]), oT.b, reads=[oT.b])

        def run_interleaved(gens):
            gens = [g for g in gens if g is not None]
            while gens:
                for g in list(gens):
                    try:
                        next(g)
                    except StopIteration:
                        gens.remove(g)

        import os as _os
        _st = _os.environ.get("B1_STAGES", "isa")
        _nq = int(_os.environ.get("B1_NQ", NB))
        if _st != "isa" or _nq != NB:
            for qb in range(_nq):
                idx_stage(qb)
                if "s" in _st:
                    run_interleaved([select_stage(qb)])
                if "a" in _st:
                    run_interleaved([attn_stage(qb)])
        else:
            idx_stage(0)
            run_interleaved([select_stage(0)])
            if NB > 1:
                idx_stage(1)
            for qb in range(NB):
                run_interleaved([attn_stage(qb), select_stage(qb + 1) if qb + 1 < NB else None])
                if qb + 2 < NB:
                    idx_stage(qb + 2)
        P.end_phase()
        pb1.close()
        resid.close()
        if stop_after == "B1":
            break

        pb2 = ExitStack()
        ps = T(pb2.enter_context(nc.psum_tensor(f"psC{l}", [128, 8, 512], F32)), "psC")
        pb = [Buf(f"pbC{i}", excl=True) for i in range(8)]
        psT = ps.t[:, 0, :].bitcast(BF16)
        Wpa = sbuf(pb2, f"Wpa{l}", [128, 4, D], BF16)
        Wpb = sbuf(pb2, f"Wpb{l}", [128, 8, D], BF16)
        Wo = sbuf(pb2, f"Wo{l}", [128, 8, D], BF16)
        Wple = sbuf(pb2, f"Wple{l}", [128, 2, D], BF16)
        Wpg = sbuf(pb2, f"Wpg{l}", [128, 8, D], BF16)
        wst = [sbuf(pb2, f"wstB{l}_{i}", [128, D], F32) for i in range(2)]
        ggla_s = sbuf(pb2, f"ggla{l}", [128, 2], F32)
        gpp_s = sbuf(pb2, f"gpps{l}", [128, 8], F32)
        gpost_t = sbuf(pb2, f"gpost{l}", [128, D], F32)
        gpp_t = sbuf(pb2, f"gppt{l}", [128, D], F32)
        ld(ggla_s, ggla_s.t[:], gglaT[l])
        ld(gpp_s, gpp_s.t[:], gppT[l])
        ld(gpost_t, gpost_t.t[:], g_post[l].partition_broadcast(128))
        ld(gpp_t, gpp_t.t[:], g_pp[l].partition_broadcast(128))
        ci = 0
        for (Wt, src, nk, sc) in ((Wpa, w_pa, 4, None), (Wpb, w_pb, 8, "gla"), (Wo, w_o, 8, None), (Wple, w_ple, 2, None), (Wpg, w_pg, 8, "gpp")):
            for k in range(nk):
                st = wst[ci % 2]
                P.dma("sp", lambda e, st=st, src=src, k=k: e.dma_start(out=st.t[:], in_=src[l, k * 128:(k + 1) * 128, :]), st.b, writes=[st.b])
                if sc is None:
                    if ci % 2 == 0:
                        P.op("dve", lambda e, st=st, Wt=Wt, k=k: e.tensor_copy(out=Wt.t[:, k, :], in_=st.t[:]), reads=[st.b], writes=[Wt.b])
                    else:
                        P.op("act", lambda e, st=st, Wt=Wt, k=k: e.copy(out=Wt.t[:, k, :], in_=st.t[:]), reads=[st.b], writes=[Wt.b])
                else:
                    scol = ggla_s.t[:, (k % 2):(k % 2) + 1] if sc == "gla" else gpp_s.t[:, k:k + 1]
                    sbuf_ = ggla_s if sc == "gla" else gpp_s
                    P.op("dve", lambda e, st=st, Wt=Wt, k=k, scol=scol: e.tensor_scalar(out=Wt.t[:, k, :], in0=st.t[:], scalar1=scol, scalar2=None, op0=ALU.mult),
                         reads=[st.b, sbuf_.b], writes=[Wt.b])
                ci += 1

        S_f = sbuf(pb2, f"Sf{l}", [128, 4, 256], F32)
        S_b = sbuf(pb2, f"Sb{l}", [128, 4, 256], BF16)
        P.op("dve", lambda e: e.memset(S_f.t[:], 0.0), writes=[S_f.b])
        P.op("pool", lambda e: e.memset(S_b.t[:], 0.0), writes=[S_b.b])
        trib = sbuf(pb2, f"trib{l}", [128, 128], F32)
        P.op("act", lambda e: e.copy(out=trib.t[:], in_=trif.t[:]), reads=[trif.b], writes=[trib.b])

        def dbl(name, shape, dt):
            return [sbuf(pb2, f"{name}{l}_{i}", shape, dt) for i in range(2)]

        qgB = dbl("qgB", [128, 4, 128], BF16)
        kgB = dbl("kgB", [128, 4, 128], BF16)
        klB = dbl("klB", [128, 512], BF16)
        vbB = dbl("vbB", [128, 1024], BF16)
        sgbB = dbl("sgbB", [128, 1024], F32)
        smaB = dbl("smaB", [128, 1024], F32)
        smbB = dbl("smbB", [128, 1024], F32)
        oaB = dbl("oaB", [128, 4, 128], BF16)
        xB = dbl("xB", [128, D], F32)
        pB = dbl("pB", [128, 256], F32)
        ATm = sbuf(pb2, f"ATm{l}", [128, 4, 128], BF16)
        junk2 = sbuf(pb2, f"junk2{l}", [128, D], BF16)
        ssq4 = sbuf(pb2, f"ssq4{l}", [128, 4], F32)
        rs4 = sbuf(pb2, f"rs4{l}", [128, 4], F32)
        ob = sbuf(pb2, f"ob{l}", [128, D], BF16)
        obT = sbuf(pb2, f"obT{l}", [128, 8, 128], BF16)
        y1 = sbuf(pb2, f"y1{l}", [128, D], F32)
        y2 = sbuf(pb2, f"y2{l}", [128, D], F32)
        ybf = sbuf(pb2, f"ybf{l}", [128, D], BF16)
        yT = sbuf(pb2, f"yT{l}", [128, 8, 128], BF16)
        ssq2 = sbuf(pb2, f"ssq2{l}", [128, 2], F32)
        ssq1 = sbuf(pb2, f"ssq1{l}", [128, 1], F32)
        rs1 = sbuf(pb2, f"rs1{l}", [128, 1], F32)
        t1 = sbuf(pb2, f"t1{l}", [128, D], F32)
        x1 = sbuf(pb2, f"x1{l}", [128, D], F32)
        rbf = sbuf(pb2, f"rbf{l}", [128, D], BF16)
        rT = sbuf(pb2, f"rT{l}", [128, 8, 128], BF16)
        pbf = sbuf(pb2, f"pbf{l}", [128, 256], BF16)
        pT = sbuf(pb2, f"pT{l}", [128, 2, 128], BF16)
        sg = sbuf(pb2, f"sg{l}", [128, D], F32)
        ee = sbuf(pb2, f"ee{l}", [128, D], F32)
        xo = dbl("xo", [128, D], F32)

        def transpose8(src_T, dst_T, n=8):
            for k in range(n):
                P.op("pe", lambda e, k=k: e.transpose(out=psT[:, k * 128:(k + 1) * 128], in_=src_T.t[:, k * 128:(k + 1) * 128], identity=identb.t[:]),
                     reads=[src_T.b, identb.b], writes=[pb[0]])
            P.op("act", lambda e: e.copy(out=dst_T.t[:], in_=psT[:, 0:n * 128].rearrange("p (k t) -> p k t", k=n)), reads=[pb[0]], writes=[dst_T.b])

        def mm_tok(lhs_T, W, nk, banks):
            for hf in range(2):
                for k in range(nk):
                    P.op("pe", lambda e, k=k, hf=hf: e.matmul(out=ps.t[:, banks[hf], :], lhsT=lhs_T.t[:, k, :], rhs=W.t[:, k, hf * 512:(hf + 1) * 512], start=(k == 0), stop=(k == nk - 1)),
                         reads=[lhs_T.b, W.b], writes=[pb[banks[hf]]])

        def sumsq_psum(banks, dst_T):
            for hf in range(2):
                P.op("act", lambda e, hf=hf: e.activation(out=junk2.t[:, hf * 512:(hf + 1) * 512], in_=ps.t[:, banks[hf], :], func=AF.Square, accum_out=ssq2.t[:, hf:hf + 1]),
                     reads=[pb[banks[hf]]], writes=[junk2.b, ssq2.b])
            P.op("dve", lambda e: e.tensor_tensor(out=dst_T.t[:], in0=ssq2.t[:, 0:1], in1=ssq2.t[:, 1:2], op=ALU.add), reads=[ssq2.b], writes=[dst_T.b])

        for b in range(NB):
            i2 = b % 2
            qg_, kg_, kl_, vb_, sgb_, sma_, smb_, oa_, x_, p_ = (qgB[i2], kgB[i2], klB[i2], vbB[i2], sgbB[i2], smaB[i2], smbB[i2], oaB[i2], xB[i2], pB[i2])
            ld(qg_, qg_.t[:], s_qgT[b])
            ld(kg_, kg_.t[:], s_kgT[b])
            ld(kl_, kl_.t[:], s_kl[b])
            ld(vb_, vb_.t[:], s_vb[b])
            ld(sgb_, sgb_.t[:], s_sgb[b])
            ld(sma_, sma_.t[:], s_sma[b])
            ld(smb_, smb_.t[:], s_smb[b])
            ld(oa_, oa_.t[:], s_oagT[b])
            ld(x_, x_.t[:], x_src[b * 128:(b + 1) * 128, :])
            ld(p_, p_.t[:], p_in[l, b * 128:(b + 1) * 128, :])
            for h in range(4):
                P.op("pe", lambda e, h=h: e.matmul(out=ps.t[:, 0, h * 128:(h + 1) * 128], lhsT=kg_.t[:, h, :], rhs=qg_.t[:, h, :], start=True, stop=True),
                     reads=[kg_.b, qg_.b], writes=[pb[0]])
            P.op("dve", lambda e: e.tensor_tensor(out=ATm.t[:], in0=ps.t[:, 0, :].rearrange("p (h t) -> p h t", h=4), in1=trib.t[:].unsqueeze(1).to_broadcast([128, 4, 128]), op=ALU.mult),
                 reads=[pb[0], trib.b], writes=[ATm.b])
            for h in range(4):
                bank = 1 + h // 2
                cs = slice((h % 2) * 256, (h % 2 + 1) * 256)
                P.op("pe", lambda e, h=h, bank=bank, cs=cs: e.matmul(out=ps.t[:, bank, cs], lhsT=ATm.t[:, h, :], rhs=vb_.t[:, h * 256:(h + 1) * 256], start=True, stop=False),
                     reads=[ATm.b, vb_.b], writes=[pb[bank]])
                P.op("pe", lambda e, h=h, bank=bank, cs=cs: e.matmul(out=ps.t[:, bank, cs], lhsT=qg_.t[:, h, :], rhs=S_b.t[:, h, :], start=False, stop=True),
                     reads=[qg_.b, S_b.b], writes=[pb[bank]])
            for h in range(4):
                bank = 3 + h // 2
                cs = slice((h % 2) * 256, (h % 2 + 1) * 256)
                P.op("pe", lambda e, h=h, bank=bank, cs=cs: e.matmul(out=ps.t[:, bank, cs], lhsT=kl_.t[:, h * 128:(h + 1) * 128], rhs=vb_.t[:, h * 256:(h + 1) * 256], start=True, stop=True),
                     reads=[kl_.b, vb_.b], writes=[pb[bank]])
            for h in range(4):
                bank = 3 + h // 2
                cs = slice((h % 2) * 256, (h % 2 + 1) * 256)
                P.op("dve", lambda e, h=h, bank=bank, cs=cs: e.scalar_tensor_tensor(out=S_f.t[:, h, :], in0=S_f.t[:, h, :], scalar=eGl.t[:, b, h:h + 1], in1=ps.t[:, bank, cs], op0=ALU.mult, op1=ALU.add),
                     reads=[S_f.b, eGl.b, pb[bank]], writes=[S_f.b])
            P.op("pool", lambda e: e.tensor_copy(out=S_b.t[:], in_=S_f.t[:]), reads=[S_f.b], writes=[S_b.b])
            for h in range(4):
                bank = 1 + h // 2
                cs = slice((h % 2) * 256, (h % 2 + 1) * 256)
                P.op("act", lambda e, h=h, bank=bank, cs=cs: e.activation(out=junk2.t[:, h * 256:(h + 1) * 256], in_=ps.t[:, bank, cs], func=AF.Square, accum_out=ssq4.t[:, h:h + 1]),
                     reads=[pb[bank]], writes=[junk2.b, ssq4.b])
            rsqrt_col(ssq4, ssq4.t[:], rs4, rs4.t[:], 4, 1.0 / 256, ncols=4)
            for h in range(4):
                bank = 1 + h // 2
                cs = slice((h % 2) * 256, (h % 2 + 1) * 256)
                P.op("dve", lambda e, h=h, bank=bank, cs=cs: e.scalar_tensor_tensor(out=ob.t[:, h * 256:(h + 1) * 256], in0=ps.t[:, bank, cs], scalar=rs4.t[:, h:h + 1], in1=sgb_.t[:, h * 256:(h + 1) * 256], op0=ALU.mult, op1=ALU.mult),
                     reads=[pb[bank], rs4.b, sgb_.b], writes=[ob.b])
            transpose8(ob, obT)
            mm_tok(oa_, Wpa, 4, (1, 2))
            mm_tok(obT, Wpb, 8, (3, 4))
            for hf in range(2):
                cs = slice(hf * 512, (hf + 1) * 512)
                P.op("dve", lambda e, hf=hf, cs=cs: e.tensor_tensor(out=y1.t[:, cs], in0=ps.t[:, 1 + hf, :], in1=sma_.t[:, cs], op=ALU.mult), reads=[pb[1 + hf], sma_.b], writes=[y1.b])
                P.op("dve", lambda e, hf=hf, cs=cs: e.tensor_tensor(out=y2.t[:, cs], in0=ps.t[:, 3 + hf, :], in1=smb_.t[:, cs], op=ALU.mult), reads=[pb[3 + hf], smb_.b], writes=[y2.b])
            P.op("pool", lambda e: e.tensor_tensor(out=ybf.t[:], in0=y1.t[:], in1=y2.t[:], op=ALU.add), reads=[y1.b, y2.b], writes=[ybf.b])
            transpose8(ybf, yT)
            mm_tok(yT, Wo, 8, (5, 6))
            sumsq_psum((5, 6), ssq1)
            rsqrt_col(ssq1, ssq1.t[:], rs1, rs1.t[:], 1, 1.0 / D)
            for hf in range(2):
                cs = slice(hf * 512, (hf + 1) * 512)
                P.op("dve", lambda e, hf=hf, cs=cs: e.scalar_tensor_tensor(out=t1.t[:, cs], in0=ps.t[:, 5 + hf, :], scalar=rs1.t[:], in1=gpost_t.t[:, cs], op0=ALU.mult, op1=ALU.mult),
                     reads=[pb[5 + hf], rs1.b, gpost_t.b], writes=[t1.b])
            P.op("pool", lambda e: e.tensor_tensor(out=x1.t[:], in0=t1.t[:], in1=x_.t[:], op=ALU.add), reads=[t1.b, x_.b], writes=[x1.b])
            P.op("act", lambda e: e.activation(out=junk2.t[:], in_=x1.t[:], func=AF.Square, accum_out=ssq1.t[:]), reads=[x1.b], writes=[junk2.b, ssq1.b])
            rsqrt_col(ssq1, ssq1.t[:], rs1, rs1.t[:], 1, 1.0 / D)
            P.op("dve", lambda e: e.tensor_scalar(out=rbf.t[:], in0=x1.t[:], scalar1=rs1.t[:], scalar2=None, op0=ALU.mult), reads=[x1.b, rs1.b], writes=[rbf.b])
            transpose8(rbf, rT)
            mm_tok(rT, Wpg, 8, (5, 6))
            P.op("act", lambda e: e.copy(out=pbf.t[:], in_=p_.t[:]), reads=[p_.b], writes=[pbf.b])
            transpose8(pbf, pT, n=2)
            mm_tok(pT, Wple, 2, (1, 2))
            for hf in range(2):
                cs = slice(hf * 512, (hf + 1) * 512)
                P.op("act", lambda e, hf=hf, cs=cs: e.activation(out=sg.t[:, cs], in_=ps.t[:, 5 + hf, :], func=AF.Sigmoid), reads=[pb[5 + hf]], writes=[sg.b])
                P.op("dve", lambda e, hf=hf, cs=cs: e.tensor_tensor(out=ee.t[:, cs], in0=ps.t[:, 1 + hf, :], in1=sg.t[:, cs], op=ALU.mult), reads=[pb[1 + hf], sg.b], writes=[ee.b])
            P.op("act", lambda e: e.activation(out=junk2.t[:], in_=ee.t[:], func=AF.Square, accum_out=ssq1.t[:]), reads=[ee.b], writes=[junk2.b, ssq1.b])
            rsqrt_col(ssq1, ssq1.t[:], rs1, rs1.t[:], 1, 1.0 / D)
            P.op("dve", lambda e: e.scalar_tensor_tensor(out=t1.t[:], in0=ee.t[:], scalar=rs1.t[:], in1=gpp_t.t[:], op0=ALU.mult, op1=ALU.mult),
                 reads=[ee.b, rs1.b, gpp_t.b], writes=[t1.b])
            xo_ = xo[i2]
            P.op("pool", lambda e, xo_=xo_: e.tensor_tensor(out=xo_.t[:], in0=t1.t[:], in1=x1.t[:], op=ALU.add), reads=[t1.b, x1.b], writes=[xo_.b])
            P.dma("pool", lambda e, xo_=xo_, b=b: e.dma_start(out=x_dst[b * 128:(b + 1) * 128, :], in_=xo_.t[:]), xo_.b, reads=[xo_.b])
        P.end_phase()
        pb2.close()

    glob.close()
    P.close()
    return nc, P


_CACHE = {}


def _consts(NIT=16):
    j = np.arange(128)[:, None]
    i = np.arange(128)[None, :]
    return {
        "c_ident": np.eye(128, dtype=np.float32),
        "c_tri": (j <= i).astype(np.float32),
        "c_triu": (j > i).astype(np.float32),
        "c_cmask": np.where(i <= j, 0.0, NEG).astype(np.float32),
        "c_pw": np.broadcast_to((0.5 ** (np.arange(NIT) + 1)).astype(np.float32), (128, NIT)).copy(),
    }


def make_in_map(xb, pb_, wts):
    m = {"x": np.ascontiguousarray(xb), "p": np.ascontiguousarray(pb_)}
    m.update(wts)
    return m


def prep_weights(g_pre, w_in, w_gate_up, b_gate, g_gla_head, w_proj_a, w_proj_b, w_out, g_post, w_ple,
                 w_ple_gate, g_ple_pre, g_ple_post):
    dep = g_pre.shape[0]
    c = lambda a: np.ascontiguousarray(np.asarray(a, dtype=np.float32))
    w = {
        "w_in": c(w_in), "gpreT": c(np.asarray(g_pre).reshape(dep, 8, 128).transpose(0, 2, 1)),
        "w_gate_up": c(w_gate_up), "b_gate": c(b_gate),
        "gglaT": c(np.asarray(g_gla_head).reshape(dep, 2, 128).transpose(0, 2, 1)),
        "w_proj_a": c(w_proj_a), "w_proj_b": c(w_proj_b), "w_out": c(w_out), "g_post": c(g_post),
        "w_ple": c(w_ple), "w_ple_gate": c(w_ple_gate),
        "gppT": c(np.asarray(g_ple_pre).reshape(dep, 8, 128).transpose(0, 2, 1)), "g_ple_post": c(g_ple_post),
    }
    w.update(_consts())
    return w


def kernel(x, p, g_pre, w_in, w_gate_up, b_gate, g_gla_head, w_proj_a, w_proj_b, w_out, g_post, w_ple,
           w_ple_gate, g_ple_pre, g_ple_post):
    x = np.asarray(x, dtype=np.float32)
    p = np.asarray(p, dtype=np.float32)
    B, L, _ = x.shape
    if L not in _CACHE:
        _CACHE[L] = build(L)[0]
    nc = _CACHE[L]
    wts = prep_weights(g_pre, w_in, w_gate_up, b_gate, g_gla_head, w_proj_a, w_proj_b, w_out, g_post, w_ple,
                       w_ple_gate, g_ple_pre, g_ple_post)
    in_maps = [make_in_map(x[b], p[:, b], wts) for b in range(B)]
    res = run_bass_kernel_spmd(nc, in_maps, core_ids=list(range(B)))
    return np.stack([np.asarray(r["y"], dtype=np.float32) for r in res.results], axis=0)
```

```python
from contextlib import ExitStack
import math
import types

import numpy as np
import concourse.bass as bass
import concourse.mybir as mybir
from concourse.bass_utils import run_bass_kernel_spmd

F32 = mybir.dt.float32
BF16 = mybir.dt.bfloat16
ALU = mybir.AluOpType
AF = mybir.ActivationFunctionType
AX = mybir.AxisListType

D = 1024
DEPTH = 2
INW = 6872
EPS = 1e-6
NEG = -1.0e30
ENGS = ("pe", "act", "dve", "pool", "sp")

C_QA, C_KA, C_VA, C_QI, C_KI, C_WI, C_GA = 0, 512, 576, 640, 1152, 1216, 1224
C_QB, C_KB, C_VB, C_GD, C_GB, C_MA, C_MB = 1736, 2248, 2760, 3784, 3800, 4824, 5848


class Buf:
    __slots__ = ("name", "w", "r", "dsem", "dcnt", "excl")

    def __init__(self, name, excl=False):
        self.name = name
        self.excl = excl
        self.w = None
        self.r = []
        self.dsem = None
        self.dcnt = 0


def _freeze(fn):
    if fn.__closure__ is None:
        return fn
    cells = []
    for c in fn.__closure__:
        try:
            cells.append(types.CellType(c.cell_contents))
        except ValueError:
            cells.append(c)
    g = types.FunctionType(fn.__code__, fn.__globals__, fn.__name__, fn.__defaults__, tuple(cells))
    g.__kwdefaults__ = fn.__kwdefaults__
    return g


class Prog:
    def __init__(self, nc):
        self.nc = nc
        self.es = ExitStack()
        self.ops = {e: [] for e in ENGS}
        self.sem = {}
        self.cnt = {}
        self.waited = {e: {} for e in ENGS}
        self.semobj = {}
        self.dma_final = {}
        self.dpool = {}
        self.dval = {}
        self.dbufs = []
        self.nsem = 0
        self.phase_id = 0
        self.total = 0
        import os as _os
        self.maxops = int(_os.environ.get("PROG_MAXOPS", 10 ** 9))
        self.nrec = 0
        self.last_desc = None
        self._new_engine_sems()

    def _alloc_sem(self, name):
        s = self.es.enter_context(self.nc.semaphore(name))
        self.semobj[name] = s
        self.nsem += 1
        return name

    def _new_engine_sems(self):
        for e in ENGS:
            self.sem[e] = self._alloc_sem(f"p{self.phase_id}_{e}")
            self.cnt[e] = 0

    def _wait(self, eng, tok):
        if tok is None:
            return
        key, val = tok
        if eng == "pe" and key == self.sem["pe"]:
            return
        w = self.waited[eng]
        if w.get(key, 0) >= val:
            return
        w[key] = val
        self.ops[eng].append(("wait", key, val))

    def _deps(self, eng, reads, writes):
        for b in reads:
            self._wait(eng, b.w)
            if b.excl:
                for t in b.r:
                    self._wait(eng, t)
        for b in writes:
            self._wait(eng, b.w)
            for t in b.r:
                self._wait(eng, t)

    def _mark(self, tok, reads, writes):
        for b in reads:
            b.r.append(tok)
            if len(b.r) > 24:
                b.r = b.r[-24:] if False else b.r
        for b in writes:
            b.w = tok
            b.r = []

    def op(self, eng, fn, reads=(), writes=()):
        if self.nrec >= self.maxops:
            return None
        self.nrec += 1
        self.last_desc = (eng, fn.__code__.co_firstlineno, [b.name for b in reads], [b.name for b in writes])
        self._deps(eng, reads, writes)
        self.cnt[eng] += 1
        tok = (self.sem[eng], self.cnt[eng])
        self.ops[eng].append(("op", _freeze(fn), self.sem[eng], 1, self.cnt[eng]))
        self._mark(tok, reads, writes)
        return tok

    def dma(self, eng, fn, sb, reads=(), writes=()):
        if self.nrec >= self.maxops:
            return None
        self.nrec += 1
        self.last_desc = ("dma-" + eng, fn.__code__.co_firstlineno, [b.name for b in reads], [b.name for b in writes])
        self._deps(eng, reads, writes)
        if sb.dsem is None:
            sb.dsem = {}
        if eng not in sb.dsem:
            fl = self.dpool.setdefault(eng, [])
            if fl:
                sb.dsem[eng] = fl.pop()
            else:
                sb.dsem[eng] = self._alloc_sem(f"d{eng}{self.nsem}")
                self.dval[sb.dsem[eng]] = 0
            self.dbufs.append((sb, eng))
        key = sb.dsem[eng]
        self.dval[key] += 16
        tok = (key, self.dval[key])
        self.dma_final[key] = self.dval[key]
        self.ops[eng].append(("op", _freeze(fn), key, 16, 0))
        self._mark(tok, reads, writes)
        return tok

    def barrier(self):
        toks = [(self.sem[e], self.cnt[e]) for e in ENGS if self.cnt[e] > 0]
        toks += list(self.dma_final.items())
        for e in ENGS:
            for key, val in toks:
                if key == self.sem[e]:
                    continue
                w = self.waited[e]
                if w.get(key, 0) >= val:
                    continue
                w[key] = val
                self.ops[e].append(("wait", key, val))

    def emit(self):
        nc = self.nc
        ops = self.ops
        semobj = self.semobj

        engkeys = {self.sem[e]: e for e in ENGS}
        waited_vals = {k: set() for k in engkeys}
        for e in ENGS:
            for o in ops[e]:
                if o[0] == "wait" and o[1] in engkeys:
                    waited_vals[o[1]].add(o[2])
        if not hasattr(self, "newcnt"):
            self.newcnt = {k: 0 for k in engkeys}
            self.vmap = {}
        for k, e in engkeys.items():
            for o in ops[e]:
                if o[0] == "op" and o[2] == k and o[4] in waited_vals[k]:
                    self.newcnt[k] += 1
                    self.vmap[(k, o[4])] = self.newcnt[k]
        vmap = self.vmap

        def replay(handle, lst):
            for o in lst:
                if o[0] == "wait":
                    if o[1] in engkeys:
                        handle.wait_ge(semobj[o[1]], vmap[(o[1], o[2])])
                    else:
                        handle.wait_ge(semobj[o[1]], o[2])
                else:
                    ins = o[1](handle)
                    if o[2] in engkeys:
                        if (o[2], o[4]) in vmap:
                            ins.then_inc(semobj[o[2]], 1)
                    else:
                        ins.then_inc(semobj[o[2]], o[3])

        with nc.Block() as blk:
            if ops["pe"]:
                blk.tensor(lambda e: replay(e, ops["pe"]))
            if ops["act"]:
                blk.scalar(lambda e: replay(e, ops["act"]))
            if ops["dve"]:
                blk.vector(lambda e: replay(e, ops["dve"]))
            if ops["pool"]:
                blk.gpsimd(lambda e: replay(e, ops["pool"]))
            if ops["sp"]:
                blk.sync(lambda e: replay(e, ops["sp"]))
        n = sum(len(v) for v in ops.values())
        self.total += n
        self.ops = {e: [] for e in ENGS}
        return n

    def end_phase(self):
        self.barrier()
        n = self.emit()
        self.phase_id += 1
        for b, eng in self.dbufs:
            self.dpool[eng].append(b.dsem.pop(eng))
        self.dbufs = []
        return n

    def close(self):
        self.es.close()


class T:
    def __init__(self, t, name):
        self.t = t
        self.b = Buf(name)


def build(L, nlayers=DEPTH, dbg=False, stop_after=None):
    NB = L // 128
    NSB = L // 512
    TOPK = min(256, L // 4)
    NIT = 16
    nc = bass.Bass("TRN2", target_bir_lowering=False)
    P = Prog(nc)

    def dram(name, shape, dt, kind):
        return nc.dram_tensor(name, shape, dt, kind=kind).ap()

    SCR = "ExternalOutput" if dbg else "Internal"
    x_in = dram("x", [L, D], F32, "ExternalInput")
    p_in = dram("p", [DEPTH, L, 256], F32, "ExternalInput")
    w_in = dram("w_in", [DEPTH, D, INW], F32, "ExternalInput")
    gpreT = dram("gpreT", [DEPTH, 128, 8], F32, "ExternalInput")
    wgu = dram("w_gate_up", [DEPTH, 16, 512], F32, "ExternalInput")
    bgate = dram("b_gate", [DEPTH, 512], F32, "ExternalInput")
    gglaT = dram("gglaT", [DEPTH, 128, 2], F32, "ExternalInput")
    w_pa = dram("w_proj_a", [DEPTH, 512, D], F32, "ExternalInput")
    w_pb = dram("w_proj_b", [DEPTH, D, D], F32, "ExternalInput")
    w_o = dram("w_out", [DEPTH, D, D], F32, "ExternalInput")
    g_post = dram("g_post", [DEPTH, D], F32, "ExternalInput")
    w_ple = dram("w_ple", [DEPTH, 256, D], F32, "ExternalInput")
    w_pg = dram("w_ple_gate", [DEPTH, D, D], F32, "ExternalInput")
    gppT = dram("gppT", [DEPTH, 128, 8], F32, "ExternalInput")
    g_pp = dram("g_ple_post", [DEPTH, D], F32, "ExternalInput")
    c_ident = dram("c_ident", [128, 128], F32, "ExternalInput")
    c_tri = dram("c_tri", [128, 128], F32, "ExternalInput")
    c_triu = dram("c_triu", [128, 128], F32, "ExternalInput")
    c_cmask = dram("c_cmask", [128, 128], F32, "ExternalInput")
    c_pw = dram("c_pw", [128, NIT], F32, "ExternalInput")
    y_out = dram("y", [L, D], F32, "ExternalOutput")

    s_qiT = dram("s_qiT", [NB, 128, 4, 128], BF16, SCR)
    s_qaT = dram("s_qaT", [NB, 128, 4, 128], BF16, SCR)
    s_qgT = dram("s_qgT", [NB, 128, 4, 128], BF16, SCR)
    s_kgT = dram("s_kgT", [NB, 128, 4, 128], BF16, SCR)
    s_kl = dram("s_kl", [NB, 128, 512], BF16, SCR)
    s_vb = dram("s_vb", [NB, 128, 1024], BF16, SCR)
    s_sga = dram("s_sga", [NB, 128, 512], F32, SCR)
    s_sgb = dram("s_sgb", [NB, 128, 1024], F32, SCR)
    s_sma = dram("s_sma", [NB, 128, 1024], F32, SCR)
    s_smb = dram("s_smb", [NB, 128, 1024], F32, SCR)
    s_oagT = dram("s_oagT", [NB, 128, 4, 128], BF16, SCR)
    s_x1 = dram("s_x1", [L, D], F32, SCR)

    glob = ExitStack()

    def sbuf(es, name, shape, dt):
        return T(es.enter_context(nc.sbuf_tensor(name, shape, dt)), name)

    identf = sbuf(glob, "identf", [128, 128], F32)
    identb = sbuf(glob, "identb", [128, 128], BF16)
    trif = sbuf(glob, "trif", [128, 128], F32)
    triuf = sbuf(glob, "triuf", [128, 128], F32)
    cmask = sbuf(glob, "cmask", [128, 128], F32)
    pw = sbuf(glob, "pw", [128, NIT], F32)
    neghalf = sbuf(glob, "neghalf", [128, 8], F32)
    onesrow = sbuf(glob, "onesrow", [1, 128], F32)
    eGl = sbuf(glob, "eGl", [128, NB, 4], F32)

    ld = lambda dst_T, dst_ap, src_ap: P.dma("sp", lambda e: e.dma_start(out=dst_ap, in_=src_ap), dst_T.b, writes=[dst_T.b])

    ld(identf, identf.t[:], c_ident)
    ld(trif, trif.t[:], c_tri)
    ld(triuf, triuf.t[:], c_triu)
    ld(cmask, cmask.t[:], c_cmask)
    ld(pw, pw.t[:], c_pw)
    P.op("dve", lambda e: e.tensor_copy(out=identb.t[:], in_=identf.t[:]), reads=[identf.b], writes=[identb.b])
    P.op("dve", lambda e: e.memset(neghalf.t[:], -0.5), writes=[neghalf.b])
    P.op("dve", lambda e: e.memset(onesrow.t[:], 1.0), writes=[onesrow.b])

    def rsqrt_col(src_T, src_ap, dst_T, dst_ap, n, inv_n, ncols=1):
        P.op("dve", lambda e: e.tensor_scalar(out=dst_ap, in0=src_ap, scalar1=inv_n, scalar2=EPS, op0=ALU.mult, op1=ALU.add),
             reads=[src_T.b], writes=[dst_T.b])
        P.op("pool", lambda e: e.tensor_tensor(out=dst_ap, in0=dst_ap, in1=neghalf.t[:, 0:ncols], op=ALU.pow),
             reads=[dst_T.b, neghalf.b], writes=[dst_T.b])

    for l in range(nlayers):
        x_src = x_in if l == 0 else s_x1
        x_dst = s_x1 if l < nlayers - 1 else y_out
        if nlayers == 1:
            x_dst = y_out

        resid = ExitStack()
        kiT = sbuf(resid, f"kiT{l}", [128, L], BF16)
        kaT = sbuf(resid, f"kaT{l}", [128, L], BF16)
        va = sbuf(resid, f"va{l}", [128, NB, 65], BF16)
        wi = sbuf(resid, f"wi{l}", [128, NB, 8], F32)

        import os as _os
        for cpass in [int(c) for c in _os.environ.get('A_PASSES', '01')]:
            pa = ExitStack()
            if cpass == 0:
                srcs = [(C_QA, 512), (C_QI, 512), (C_KA, 64), (C_KA, 64), (C_KI, 64), (C_KI, 64),
                        (C_QB, 512), (C_KB, 512), (C_GD, 16), (C_VA, 64), (C_WI, 8), (C_VB, 1024)]
            else:
                srcs = [(C_GA, 512), (C_GB, 1024), (C_MA, 1024), (C_MB, 1024)]
            offs = []
            o = 0
            for (c0, w) in srcs:
                offs.append(o)
                o += w
            WCOLS = o
            Wb = sbuf(pa, f"Wb{l}_{cpass}", [128, 8, WCOLS], BF16)
            wst = [sbuf(pa, f"wst{l}_{cpass}_{i}", [128, 1024], F32) for i in range(2)]
            gcol = sbuf(pa, f"gcol{l}_{cpass}", [128, 8], F32)
            ps = T(pa.enter_context(nc.psum_tensor(f"psA{l}_{cpass}", [128, 8, 512], F32)), "psA")
            pb = [Buf(f"pb{i}", excl=True) for i in range(8)]
            ld(gcol, gcol.t[:], gpreT[l])
            ci = 0
            for k in range(8):
                for (c0, w), o in zip(srcs, offs):
                    st = wst[ci % 2]
                    P.dma("sp", lambda e, st=st, k=k, c0=c0, w=w: e.dma_start(out=st.t[:, 0:w], in_=w_in[l, k * 128:(k + 1) * 128, c0:c0 + w]),
                          st.b, writes=[st.b])
                    if ci % 2 == 0:
                        P.op("dve", lambda e, st=st, k=k, o=o, w=w: e.tensor_scalar(out=Wb.t[:, k, o:o + w], in0=st.t[:, 0:w], scalar1=gcol.t[:, k:k + 1], scalar2=None, op0=ALU.mult),
                             reads=[st.b, gcol.b], writes=[Wb.b])
                    else:
                        P.op("act", lambda e, st=st, k=k, o=o, w=w: e.activation(out=Wb.t[:, k, o:o + w], in_=st.t[:, 0:w], func=AF.Copy, scale=gcol.t[:, k:k + 1]),
                             reads=[st.b, gcol.b], writes=[Wb.b])
                    ci += 1

            xs = [sbuf(pa, f"xs{l}_{cpass}_{i}", [128, D], F32) for i in range(2)]
            junk = sbuf(pa, f"junk{l}_{cpass}", [128, D], BF16)
            ssq = sbuf(pa, f"ssq{l}_{cpass}", [128, 1], F32)
            rstd = sbuf(pa, f"rstd{l}_{cpass}", [128, 1], F32)
            hn = sbuf(pa, f"hn{l}_{cpass}", [128, D], BF16)
            hT = [sbuf(pa, f"hT{l}_{cpass}_{i}", [128, 8, 512], BF16) for i in range(2)]
            psT = ps.t[:, 0, :].bitcast(BF16)
            rot = [1, 2, 3, 4]
            rc = [0]

            def nextbank():
                b = rot[rc[0] % 4]
                rc[0] += 1
                return b

            if cpass == 0:
                fm_st = {n: sbuf(pa, f"fm_{n}{l}", [128, 4, 4, 128], BF16) for n in ("qa", "qi", "qg", "kg")}
                gdT = sbuf(pa, f"gdT{l}", [16, 512], F32)
                wgu_s = sbuf(pa, f"wgu{l}", [16, 512], F32)
                bg_s = sbuf(pa, f"bg{l}", [1, 512], F32)
                e1 = sbuf(pa, f"e1_{l}", [128, 512], F32)
                sp_ = sbuf(pa, f"sp_{l}", [128, 512], F32)
                E1 = sbuf(pa, f"E1_{l}", [128, 4, 128], F32)
                E2 = sbuf(pa, f"E2_{l}", [128, 4, 128], F32)
                E3 = sbuf(pa, f"E3_{l}", [128, 512], F32)
                kl_st = [sbuf(pa, f"klst{l}_{i}", [128, 512], BF16) for i in range(2)]
                vb_st = [sbuf(pa, f"vbst{l}_{i}", [128, 1024], BF16) for i in range(2)]
                ld(wgu_s, wgu_s.t[:], wgu[l])
                ld(bg_s, bg_s.t[:], bgate[l:l + 1, :])
                P.op("pool", lambda e: e.memset(va.t[:, :, 64:65], 1.0), writes=[va.b])
                O_QA, O_QI, O_KA, O_KI, O_QB, O_KB, O_GD, O_VA, O_WI, O_VB = (
                    offs[0], offs[1], offs[2], offs[4], offs[6], offs[7], offs[8], offs[9], offs[10], offs[11])
            else:
                g_st = {n: [sbuf(pa, f"gst_{n}{l}_{i}", [128, w], F32) for i in range(2)]
                        for n, w in (("ga", 512), ("gb", 1024), ("ma", 1024), ("mb", 1024))}
                O_GA, O_GB, O_MA, O_MB = offs

            for sb in range(int(_os.environ.get('A_NSB', NSB))):
                h_T = hT[sb % 2]
                for tb in range(4):
                    blk = sb * 4 + tb
                    xb = xs[blk % 2]
                    ld(xb, xb.t[:], x_src[blk * 128:(blk + 1) * 128, :])
                    P.op("act", lambda e, xb=xb: e.activation(out=junk.t[:], in_=xb.t[:], func=AF.Square, accum_out=ssq.t[:]),
                         reads=[xb.b], writes=[junk.b, ssq.b])
                    rsqrt_col(ssq, ssq.t[:], rstd, rstd.t[:], 1, 1.0 / D)
                    P.op("dve", lambda e, xb=xb: e.tensor_scalar(out=hn.t[:], in0=xb.t[:], scalar1=rstd.t[:], scalar2=None, op0=ALU.mult),
                         reads=[xb.b, rstd.b], writes=[hn.b])
                    for k in range(8):
                        P.op("pe", lambda e, k=k: e.transpose(out=psT[:, k * 128:(k + 1) * 128], in_=hn.t[:, k * 128:(k + 1) * 128], identity=identb.t[:]),
                             reads=[hn.b, identb.b], writes=[pb[0]])
                    P.op("act", lambda e, tb=tb, h_T=h_T: e.copy(out=h_T.t[:, :, tb * 128:(tb + 1) * 128], in_=psT.rearrange("p (k t) -> p k t", k=8)),
                         reads=[pb[0]], writes=[h_T.b])

                def proj_fm(o, m, bank, ncols=512, t0=0):
                    for k in range(8):
                        P.op("pe", lambda e, k=k: e.matmul(out=ps.t[0:m, bank, 0:ncols], lhsT=Wb.t[:, k, o:o + m], rhs=h_T.t[:, k, t0:t0 + ncols], start=(k == 0), stop=(k == 7)),
                             reads=[Wb.b, h_T.b], writes=[pb[bank]])

                def proj_tm(o, n, tb, bank):
                    for k in range(8):
                        P.op("pe", lambda e, k=k: e.matmul(out=ps.t[:, bank, 0:n], lhsT=h_T.t[:, k, tb * 128:(tb + 1) * 128], rhs=Wb.t[:, k, o:o + n], start=(k == 0), stop=(k == 7)),
                             reads=[Wb.b, h_T.b], writes=[pb[bank]])

                if cpass == 0:
                    for name, o0 in (("qa", O_QA), ("qi", O_QI)):
                        st = fm_st[name]
                        for c in range(4):
                            bank = nextbank()
                            proj_fm(o0 + c * 128, 128, bank)
                            eng = "act" if c % 2 == 0 else "dve"
                            if eng == "act":
                                P.op("act", lambda e, st=st, c=c, bank=bank: e.copy(out=st.t[:, :, c, :], in_=ps.t[:, bank, :].rearrange("p (a t) -> p a t", a=4)),
                                     reads=[pb[bank]], writes=[st.b])
                            else:
                                P.op("dve", lambda e, st=st, c=c, bank=bank: e.tensor_copy(out=st.t[:, :, c, :], in_=ps.t[:, bank, :].rearrange("p (a t) -> p a t", a=4)),
                                     reads=[pb[bank]], writes=[st.b])
                    for res, o0 in ((kaT, O_KA), (kiT, O_KI)):
                        bank = nextbank()
                        proj_fm(o0, 128, bank)
                        _v = _os.environ.get("V_KA", "")
                        _sbw = 0 if _v == "same" else sb
                        if _v == "dve":
                            P.op("dve", lambda e, res=res, bank=bank: e.tensor_copy(out=res.t[:, sb * 512:(sb + 1) * 512], in_=ps.t[:, bank, :]),
                                 reads=[pb[bank]], writes=[res.b])
                        elif _v == "nodep":
                            P.op("act", lambda e, res=res, bank=bank: e.copy(out=res.t[:, sb * 512:(sb + 1) * 512], in_=ps.t[:, bank, :]),
                                 reads=[pb[bank]], writes=[])
                        else:
                            P.op("act", lambda e, res=res, bank=bank, _sbw=_sbw: e.copy(out=res.t[:, _sbw * 512:(_sbw + 1) * 512], in_=ps.t[:, bank, :]),
                                 reads=[pb[bank]], writes=[res.b])
                    bank = nextbank()
                    proj_fm(O_GD, 16, bank)
                    P.op("dve", lambda e, bank=bank: e.tensor_copy(out=gdT.t[:], in_=ps.t[0:16, bank, :]), reads=[pb[bank]], writes=[gdT.b])

                    for tb in range(4):
                        blk = sb * 4 + tb
                        tsl = slice(tb * 128, (tb + 1) * 128)
                        bank = nextbank()
                        proj_tm(O_VA, 72, tb, bank)
                        P.op("act", lambda e, bank=bank, blk=blk: e.copy(out=va.t[:, blk, 0:64], in_=ps.t[:, bank, 0:64]), reads=[pb[bank]], writes=[va.b])
                        P.op("dve", lambda e, bank=bank, blk=blk: e.tensor_scalar(out=wi.t[:, blk, :], in0=ps.t[:, bank, 64:72], scalar1=float(8 ** -0.5 * 64 ** -0.5), scalar2=None, op0=ALU.mult),
                             reads=[pb[bank]], writes=[wi.b])
                        vst = vb_st[blk % 2]
                        for hf in range(2):
                            bank = nextbank()
                            proj_tm(O_VB + hf * 512, 512, tb, bank)
                            if hf == 0:
                                P.op("act", lambda e, bank=bank, vst=vst: e.copy(out=vst.t[:, 0:512], in_=ps.t[:, bank, :]), reads=[pb[bank]], writes=[vst.b])
                            else:
                                P.op("dve", lambda e, bank=bank, vst=vst: e.tensor_copy(out=vst.t[:, 512:1024], in_=ps.t[:, bank, :]), reads=[pb[bank]], writes=[vst.b])
                        P.dma("pool", lambda e, vst=vst, blk=blk: e.dma_start(out=s_vb[blk], in_=vst.t[:]), vst.b, reads=[vst.b])
                        P.op("pe", lambda e, tsl=tsl: e.matmul(out=ps.t[:, 5, :], lhsT=gdT.t[:, tsl], rhs=wgu_s.t[:], start=True, stop=False),
                             reads=[gdT.b, wgu_s.b], writes=[pb[5]])
                        P.op("pe", lambda e: e.matmul(out=ps.t[:, 5, :], lhsT=onesrow.t[:], rhs=bg_s.t[:], start=False, stop=True),
                             reads=[onesrow.b, bg_s.b], writes=[pb[5]])
                        P.op("act", lambda e: e.activation(out=e1.t[:], in_=ps.t[:, 5, :], func=AF.Exp, scale=-1.0), reads=[pb[5]], writes=[e1.b])
                        P.op("act", lambda e: e.activation(out=sp_.t[:], in_=e1.t[:], func=AF.Ln, bias=1.0), reads=[e1.b], writes=[sp_.b])
                        for h in range(4):
                            P.op("pe", lambda e, h=h: e.matmul(out=ps.t[:, 6, h * 128:(h + 1) * 128], lhsT=sp_.t[:, h * 128:(h + 1) * 128], rhs=trif.t[:], start=True, stop=True),
                                 reads=[sp_.b, trif.b], writes=[pb[6]])
                        P.op("pe", lambda e: e.matmul(out=ps.t[:, 5, :], lhsT=triuf.t[:], rhs=sp_.t[:], start=True, stop=True),
                             reads=[triuf.b, sp_.b], writes=[pb[5]])
                        lnsc = float(math.log(128 ** -0.5))
                        P.op("act", lambda e: e.activation(out=E1.t[:], in_=ps.t[:, 6, :].rearrange("p (h t) -> p h t", h=4), func=AF.Exp, scale=-1.0 / 16, bias=lnsc),
                             reads=[pb[6]], writes=[E1.b])
                        P.op("act", lambda e: e.activation(out=E2.t[:], in_=ps.t[:, 6, :].rearrange("p (h t) -> p h t", h=4), func=AF.Exp, scale=1.0 / 16),
                             reads=[pb[6]], writes=[E2.b])
                        P.op("act", lambda e, blk=blk: e.activation(out=eGl.t[:, blk, :], in_=ps.t[:, 6, :].rearrange("p (h t) -> p h t", h=4)[:, :, 127], func=AF.Exp, scale=-1.0 / 16),
                             reads=[pb[6]], writes=[eGl.b])
                        P.op("act", lambda e: e.activation(out=E3.t[:], in_=ps.t[:, 5, :], func=AF.Exp, scale=-1.0 / 16), reads=[pb[5]], writes=[E3.b])
                        for name, o0, Eg in (("qg", O_QB, E1), ("kg", O_KB, E2)):
                            st = fm_st[name]
                            for h in range(4):
                                for k in range(8):
                                    P.op("pe", lambda e, k=k, h=h, o0=o0: e.matmul(out=ps.t[:, 7, h * 128:(h + 1) * 128], lhsT=Wb.t[:, k, o0 + h * 128:o0 + (h + 1) * 128], rhs=h_T.t[:, k, tsl], start=(k == 0), stop=(k == 7)),
                                         reads=[Wb.b, h_T.b], writes=[pb[7]])
                            P.op("dve", lambda e, st=st, Eg=Eg, tb=tb: e.tensor_tensor(out=st.t[:, tb, :, :], in0=ps.t[:, 7, :].rearrange("p (h t) -> p h t", h=4), in1=Eg.t[:], op=ALU.mult),
                                 reads=[pb[7], Eg.b], writes=[st.b])
                        bank = nextbank()
                        proj_tm(O_KB, 512, tb, bank)
                        kst = kl_st[blk % 2]
                        P.op("dve", lambda e, bank=bank, kst=kst: e.tensor_tensor(out=kst.t[:], in0=ps.t[:, bank, :], in1=E3.t[:], op=ALU.mult),
                             reads=[pb[bank], E3.b], writes=[kst.b])
                        P.dma("pool", lambda e, kst=kst, blk=blk: e.dma_start(out=s_kl[blk], in_=kst.t[:]), kst.b, reads=[kst.b])
                    for name, dst in (("qa", s_qaT), ("qi", s_qiT), ("qg", s_qgT), ("kg", s_kgT)):
                        st = fm_st[name]
                        P.dma("pool", lambda e, st=st, dst=dst: e.dma_start(out=dst[sb * 4:(sb + 1) * 4].rearrange("a p c t -> p a c t"), in_=st.t[:]),
                              st.b, reads=[st.b])
                else:
                    for tb in range(4):
                        blk = sb * 4 + tb
                        for name, o0, w, fn, dst in (("ga", O_GA, 512, AF.Silu, s_sga), ("gb", O_GB, 1024, AF.Silu, s_sgb),
                                                     ("ma", O_MA, 1024, AF.Sigmoid, s_sma), ("mb", O_MB, 1024, AF.Sigmoid, s_smb)):
                            st = g_st[name][blk % 2]
                            for hf in range(w // 512):
                                bank = nextbank()
                                proj_tm(o0 + hf * 512, 512, tb, bank)
                                P.op("act", lambda e, st=st, hf=hf, bank=bank, fn=fn: e.activation(out=st.t[:, hf * 512:(hf + 1) * 512], in_=ps.t[:, bank, :], func=fn),
                                     reads=[pb[bank]], writes=[st.b])
                            P.dma("pool", lambda e, st=st, dst=dst, blk=blk: e.dma_start(out=dst[blk], in_=st.t[:]), st.b, reads=[st.b])
            P.end_phase()
            pa.close()
        if stop_after == "A":
            resid.close()
            break

        pb1 = ExitStack()
        ps = T(pb1.enter_context(nc.psum_tensor(f"psB{l}", [128, 8, 512], F32)), "psB")
        pb = [Buf(f"pbB{i}", excl=True) for i in range(8)]
        psT = ps.t[:, 3, :].bitcast(BF16)
        scoreB = [sbuf(pb1, f"score{l}_{i}", [128, L], F32) for i in range(2)]
        maskq = sbuf(pb1, f"maskq{l}", [128, L], BF16)
        maskT = [sbuf(pb1, f"maskT{l}_{i}", [128, NB, 128], BF16) for i in range(2)]
        Rb = [sbuf(pb1, f"Rb{l}_{i}", [128, 512], BF16) for i in range(2)]
        dg = [sbuf(pb1, f"dg{l}_{i}", [128, 8, 128], BF16) for i in range(2)]
        qiB = [sbuf(pb1, f"qiB{l}_{i}", [128, 4, 128], BF16) for i in range(2)]
        qaB = [sbuf(pb1, f"qaB{l}_{i}", [128, 4, 128], BF16) for i in range(2)]
        sgaB = [sbuf(pb1, f"sgaB{l}_{i}", [128, 512], F32) for i in range(2)]
        qaEB = [sbuf(pb1, f"qaEB{l}_{i}", [128, 4, 128], BF16) for i in range(2)]
        qaOB = [sbuf(pb1, f"qaOB{l}_{i}", [128, 4, 128], BF16) for i in range(2)]
        qiEB = [sbuf(pb1, f"qiEB{l}_{i}", [128, 4, 128], BF16) for i in range(2)]
        qiOB = [sbuf(pb1, f"qiOB{l}_{i}", [128, 4, 128], BF16) for i in range(2)]
        Pt = [sbuf(pb1, f"Pt{l}_{i}", [128, 8, 128], BF16) for i in range(2)]
        lo = sbuf(pb1, f"lo{l}", [128, 1], F32)
        w0 = sbuf(pb1, f"w0{l}", [128, 1], F32)
        WH = sbuf(pb1, f"WH{l}", [128, NIT], F32)
        mid = sbuf(pb1, f"mid{l}", [128, 1], F32)
        cnt = sbuf(pb1, f"cnt{l}", [128, 1], F32)
        stp = sbuf(pb1, f"stp{l}", [128, 1], F32)
        tau = [sbuf(pb1, f"tau{l}_{i}", [128, 1], F32) for i in range(2)]
        rinv = sbuf(pb1, f"rinv{l}", [128, 8], F32)
        oan = sbuf(pb1, f"oan{l}", [128, 8, 64], F32)
        oag = sbuf(pb1, f"oag{l}", [128, 512], BF16)
        oagT = [sbuf(pb1, f"oagT{l}_{i}", [128, 4, 128], BF16) for i in range(2)]
        att_scale = float(64 ** -0.5)

        import os as _os
        PVS = int(_os.environ.get("PVS", 66))

        def idx_stage(qb):
            S = (qb + 1) * 128
            score = scoreB[qb % 2]
            qi_ = qiB[qb % 2]
            dg_ = dg[qb % 2]
            ld(qi_, qi_.t[:], s_qiT[qb])
            qa_ = qaB[qb % 2]
            ld(qa_, qa_.t[:], s_qaT[qb])
            sg_ = sgaB[qb % 2]
            ld(sg_, sg_.t[:], s_sga[qb])
            qaE_ = qaEB[qb % 2]
            qaO_ = qaOB[qb % 2]
            P.op("pool", lambda e: e.tensor_copy(out=qaE_.t[:], in_=qa_.t[:]), reads=[qa_.b], writes=[qaE_.b])
            P.op("pool", lambda e: e.memset(qaE_.t[64:128, :, :], 0.0), writes=[qaE_.b])
            P.op("pool", lambda e: e.tensor_copy(out=qaO_.t[:], in_=qa_.t[:]), reads=[qa_.b], writes=[qaO_.b])
            P.op("pool", lambda e: e.memset(qaO_.t[0:64, :, :], 0.0), writes=[qaO_.b])
            qiE_ = qiEB[qb % 2]
            qiO_ = qiOB[qb % 2]
            P.op("pool", lambda e: e.tensor_copy(out=qiE_.t[:], in_=qi_.t[:]), reads=[qi_.b], writes=[qiE_.b])
            P.op("pool", lambda e: e.memset(qiE_.t[64:128, :, :], 0.0), writes=[qiE_.b])
            P.op("pool", lambda e: e.tensor_copy(out=qiO_.t[:], in_=qi_.t[:]), reads=[qi_.b], writes=[qiO_.b])
            P.op("pool", lambda e: e.memset(qiO_.t[0:64, :, :], 0.0), writes=[qiO_.b])
            P.op("dve", lambda e: e.tensor_tensor(out=dg_.t[:], in0=identb.t[:].unsqueeze(1).to_broadcast([128, 8, 128]),
                                                   in1=wi.t[:, qb, :].unsqueeze(2).to_broadcast([128, 8, 128]), op=ALU.mult),
                 reads=[identb.b, wi.b], writes=[dg_.b])
            nt = (S + 511) // 512
            units = [(kt, h) for kt in range(nt) for h in range(8)]

            def logits(kt, h):
                k0 = kt * 512
                wd = min(512, S - k0)
                bank = h % 2
                P.op("pe", lambda e: e.matmul(out=ps.t[:, bank, 0:wd], lhsT=(qiE_ if h % 2 == 0 else qiO_).t[:, h // 2, :], rhs=kiT.t[:, k0:k0 + wd], start=True, stop=True),
                     reads=[qiE_.b, qiO_.b, kiT.b], writes=[pb[bank]])

            def relu_diag_a(kt, h):
                k0 = kt * 512
                wd = min(512, S - k0)
                bank = h % 2
                R = Rb[h % 2]
                P.op("act", lambda e: e.activation(out=R.t[:, 0:wd], in_=ps.t[:, bank, 0:wd], func=AF.Relu),
                     reads=[pb[bank]], writes=[R.b])

            def relu_diag_b(kt, h):
                k0 = kt * 512
                wd = min(512, S - k0)
                R = Rb[h % 2]
                P.op("pe", lambda e: e.matmul(out=ps.t[:, 2, 0:wd], lhsT=dg_.t[:, h, :], rhs=R.t[:, 0:wd], start=(h == 0), stop=(h == 7)),
                     reads=[dg_.b, R.b], writes=[pb[2]])
                if h == 7:
                    P.op("act", lambda e: e.copy(out=score.t[:, k0:k0 + wd], in_=ps.t[:, 2, 0:wd]), reads=[pb[2]], writes=[score.b])

            logits(*units[0])
            for ui, (kt, h) in enumerate(units):
                relu_diag_a(kt, h)
                if ui + 1 < len(units):
                    logits(*units[ui + 1])
                relu_diag_b(kt, h)
            P.op("pool", lambda e: e.tensor_tensor(out=score.t[:, qb * 128:S], in0=score.t[:, qb * 128:S], in1=cmask.t[:], op=ALU.add),
                 reads=[score.b, cmask.b], writes=[score.b])

        def select_stage(qb):
            S = (qb + 1) * 128
            score = scoreB[qb % 2]
            ta = tau[qb % 2]
            mT = maskT[qb % 2]
            if S > TOPK:
                SV = qb * 128
                P.op("dve", lambda e: e.tensor_reduce(out=lo.t[:], in_=score.t[:, 0:SV], axis=AX.X, op=ALU.min), reads=[score.b], writes=[lo.b])
                P.op("dve", lambda e: e.tensor_reduce(out=w0.t[:], in_=score.t[:, 0:S], axis=AX.X, op=ALU.max), reads=[score.b], writes=[w0.b])
                P.op("dve", lambda e: e.tensor_tensor(out=w0.t[:], in0=w0.t[:], in1=lo.t[:], op=ALU.subtract), reads=[w0.b, lo.b], writes=[w0.b])
                P.op("dve", lambda e: e.tensor_scalar(out=WH.t[:], in0=pw.t[:], scalar1=w0.t[:], scalar2=None, op0=ALU.mult), reads=[pw.b, w0.b], writes=[WH.b])
                for it in range(NIT):
                    P.op("dve", lambda e, it=it: e.tensor_tensor(out=mid.t[:], in0=lo.t[:], in1=WH.t[:, it:it + 1], op=ALU.add), reads=[lo.b, WH.b], writes=[mid.b])
                    P.op("dve", lambda e: e.tensor_scalar(out=maskq.t[:, 0:S], in0=score.t[:, 0:S], scalar1=mid.t[:], scalar2=None, op0=ALU.is_ge, op1=ALU.add, accum_out=cnt.t[:]),
                         reads=[score.b, mid.b], writes=[maskq.b, cnt.b])
                    P.op("dve", lambda e, it=it: e.tensor_scalar(out=stp.t[:], in0=cnt.t[:], scalar1=float(TOPK) - 0.5, scalar2=WH.t[:, it:it + 1], op0=ALU.is_ge, op1=ALU.mult),
                         reads=[cnt.b, WH.b], writes=[stp.b])
                    P.op("dve", lambda e: e.tensor_tensor(out=lo.t[:], in0=lo.t[:], in1=stp.t[:], op=ALU.add), reads=[lo.b, stp.b], writes=[lo.b])
                    yield
                P.op("dve", lambda e: e.tensor_copy(out=ta.t[:], in_=lo.t[:]), reads=[lo.b], writes=[ta.b])
            else:
                P.op("dve", lambda e: e.memset(ta.t[:], -1.0e29), writes=[ta.b])
            P.op("dve", lambda e: e.tensor_scalar(out=maskq.t[:, 0:S], in0=score.t[:, 0:S], scalar1=ta.t[:], scalar2=None, op0=ALU.is_ge),
                 reads=[score.b, ta.b], writes=[maskq.b])
            for g0 in range(0, qb + 1, 8):
                n = min(8, qb + 1 - g0)
                for j in range(n):
                    kt = g0 + j
                    P.op("pe", lambda e, j=j, kt=kt: e.transpose(out=psT[:, j * 128:(j + 1) * 128], in_=maskq.t[:, kt * 128:(kt + 1) * 128], identity=identb.t[:]),
                         reads=[maskq.b, identb.b], writes=[pb[3]])
                P.op("act", lambda e, g0=g0, n=n: e.copy(out=mT.t[:, g0:g0 + n, :], in_=psT[:, 0:n * 128].rearrange("p (a t) -> p a t", a=n)),
                     reads=[pb[3]], writes=[mT.b])

        def attn_stage(qb):
            qa_ = qaB[qb % 2]
            sg_ = sgaB[qb % 2]
            mT = maskT[qb % 2]
            def scores(kt):
                for h in range(8):
                    bank = 4 + h // 4
                    P.op("pe", lambda e, h=h, bank=bank: e.matmul(out=ps.t[:, bank, (h % 4) * 128:(h % 4 + 1) * 128], lhsT=kaT.t[:, kt * 128:(kt + 1) * 128],
                                                               rhs=(qaEB[qb % 2] if h % 2 == 0 else qaOB[qb % 2]).t[:, h // 2, :], start=True, stop=True),
                         reads=[kaT.b, qaEB[qb % 2].b, qaOB[qb % 2].b], writes=[pb[bank]])

            def expmask(kt):
                Pm = Pt[kt % 2]
                for g in range(2):
                    P.op("act", lambda e, g=g: e.activation(out=Pm.t[:, g * 4:(g + 1) * 4, :], in_=ps.t[:, 4 + g, :].rearrange("p (h t) -> p h t", h=4), func=AF.Exp, scale=att_scale),
                         reads=[pb[4 + g]], writes=[Pm.b])
                P.op("dve", lambda e: e.tensor_tensor(out=Pm.t[:], in0=Pm.t[:], in1=mT.t[:, kt, :].unsqueeze(1).to_broadcast([128, 8, 128]), op=ALU.mult),
                     reads=[Pm.b, mT.b], writes=[Pm.b])

            def pvmm(kt):
                Pm = Pt[kt % 2]
                for h in range(8):
                    bank = 6 + h // 4
                    c0 = (h % 4) * PVS
                    P.op("pe", lambda e, h=h, bank=bank, c0=c0: e.matmul(out=ps.t[:, bank, c0:c0 + 65], lhsT=Pm.t[:, h, :], rhs=va.t[:, kt, 0:65],
                                                                       start=(kt == 0 and h % 4 == 0), stop=(kt == qb), skip_group_check=True),
                         reads=[Pm.b, va.b], writes=[pb[bank]])

            scores(0)
            for kt in range(qb + 1):
                expmask(kt)
                if kt + 1 <= qb:
                    scores(kt + 1)
                pvmm(kt)
                yield
            if _os.environ.get("B1_FIN", "1") == "0":
                return
            pv = ps.t[:, 6:8, 0:4 * PVS].rearrange("p b (h d) -> p b h d", d=PVS)
            P.op("dve", lambda e: e.reciprocal(out=rinv.t[:].rearrange("p (b h o) -> p b h o", b=2, o=1), in_=pv[:, :, :, 64:65]),
                 reads=[pb[6], pb[7]], writes=[rinv.b])
            for b2 in range(2):
                P.op("dve", lambda e, b2=b2: e.tensor_tensor(out=oan.t[:, b2 * 4:(b2 + 1) * 4, :], in0=pv[:, b2, :, 0:64],
                                                           in1=rinv.t[:, b2 * 4:(b2 + 1) * 4].unsqueeze(2).to_broadcast([128, 4, 64]), op=ALU.mult),
                     reads=[pb[6 + b2], rinv.b], writes=[oan.b])
            P.op("dve", lambda e: e.tensor_tensor(out=oag.t[:], in0=oan.t[:].rearrange("p h d -> p (h d)"), in1=sg_.t[:], op=ALU.mult),
                 reads=[oan.b, sg_.b], writes=[oag.b])
            oT = oagT[qb % 2]
            for c in range(4):
                P.op("pe", lambda e, c=c: e.transpose(out=psT[:, c * 128:(c + 1) * 128], in_=oag.t[:, c * 128:(c + 1) * 128], identity=identb.t[:]),
                     reads=[oag.b, identb.b], writes=[pb[3]])
            P.op("act", lambda e: e.copy(out=oT.t[:], in_=psT[:, 0:512].rearrange("p (c t) -> p c t", c=4)), reads=[pb[3]], writes=[oT.b])
            P.dma("pool", lambda e: e.dma_start(out=s_oagT[qb], in_=oT.t[:]), oT.b, reads=[oT.b])

        def run_interleaved(gens):
            gens = [g for g in gens if g is not None]
            while gens:
                for g in list(gens):
                    try:
                        next(g)
                    except StopIteration:
                        gens.remove(g)

        import os as _os
        _st = _os.environ.get("B1_STAGES", "isa")
        _nq = int(_os.environ.get("B1_NQ", NB))
        if _st != "isa" or _nq != NB:
            for qb in range(_nq):
                idx_stage(qb)
                if "s" in _st:
                    run_interleaved([select_stage(qb)])
                if "a" in _st:
                    run_interleaved([attn_stage(qb)])
        else:
            idx_stage(0)
            run_interleaved([select_stage(0)])
            if NB > 1:
                idx_stage(1)
            for qb in range(NB):
                run_interleaved([attn_stage(qb), select_stage(qb + 1) if qb + 1 < NB else None])
                if qb + 2 < NB:
                    idx_stage(qb + 2)
        P.end_phase()
        pb1.close()
        resid.close()
        if stop_after == "B1":
            break

        pb2 = ExitStack()
        ps = T(pb2.enter_context(nc.psum_tensor(f"psC{l}", [128, 8, 512], F32)), "psC")
        pb = [Buf(f"pbC{i}", excl=True) for i in range(8)]
        psT = ps.t[:, 0, :].bitcast(BF16)
        Wpa = sbuf(pb2, f"Wpa{l}", [128, 4, D], BF16)
        Wpb = sbuf(pb2, f"Wpb{l}", [128, 8, D], BF16)
        Wo = sbuf(pb2, f"Wo{l}", [128, 8, D], BF16)
        Wple = sbuf(pb2, f"Wple{l}", [128, 2, D], BF16)
        Wpg = sbuf(pb2, f"Wpg{l}", [128, 8, D], BF16)
        wst = [sbuf(pb2, f"wstB{l}_{i}", [128, D], F32) for i in range(2)]
        ggla_s = sbuf(pb2, f"ggla{l}", [128, 2], F32)
        gpp_s = sbuf(pb2, f"gpps{l}", [128, 8], F32)
        gpost_t = sbuf(pb2, f"gpost{l}", [128, D], F32)
        gpp_t = sbuf(pb2, f"gppt{l}", [128, D], F32)
        ld(ggla_s, ggla_s.t[:], gglaT[l])
        ld(gpp_s, gpp_s.t[:], gppT[l])
        ld(gpost_t, gpost_t.t[:], g_post[l].partition_broadcast(128))
        ld(gpp_t, gpp_t.t[:], g_pp[l].partition_broadcast(128))
        ci = 0
        for (Wt, src, nk, sc) in ((Wpa, w_pa, 4, None), (Wpb, w_pb, 8, "gla"), (Wo, w_o, 8, None), (Wple, w_ple, 2, None), (Wpg, w_pg, 8, "gpp")):
            for k in range(nk):
                st = wst[ci % 2]
                P.dma("sp", lambda e, st=st, src=src, k=k: e.dma_start(out=st.t[:], in_=src[l, k * 128:(k + 1) * 128, :]), st.b, writes=[st.b])
                if sc is None:
                    if ci % 2 == 0:
                        P.op("dve", lambda e, st=st, Wt=Wt, k=k: e.tensor_copy(out=Wt.t[:, k, :], in_=st.t[:]), reads=[st.b], writes=[Wt.b])
                    else:
                        P.op("act", lambda e, st=st, Wt=Wt, k=k: e.copy(out=Wt.t[:, k, :], in_=st.t[:]), reads=[st.b], writes=[Wt.b])
                else:
                    scol = ggla_s.t[:, (k % 2):(k % 2) + 1] if sc == "gla" else gpp_s.t[:, k:k + 1]
                    sbuf_ = ggla_s if sc == "gla" else gpp_s
                    P.op("dve", lambda e, st=st, Wt=Wt, k=k, scol=scol: e.tensor_scalar(out=Wt.t[:, k, :], in0=st.t[:], scalar1=scol, scalar2=None, op0=ALU.mult),
                         reads=[st.b, sbuf_.b], writes=[Wt.b])
                ci += 1

        S_f = sbuf(pb2, f"Sf{l}", [128, 4, 256], F32)
        S_b = sbuf(pb2, f"Sb{l}", [128, 4, 256], BF16)
        P.op("dve", lambda e: e.memset(S_f.t[:], 0.0), writes=[S_f.b])
        P.op("pool", lambda e: e.memset(S_b.t[:], 0.0), writes=[S_b.b])
        trib = sbuf(pb2, f"trib{l}", [128, 128], F32)
        P.op("act", lambda e: e.copy(out=trib.t[:], in_=trif.t[:]), reads=[trif.b], writes=[trib.b])

        def dbl(name, shape, dt):
            return [sbuf(pb2, f"{name}{l}_{i}", shape, dt) for i in range(2)]

        qgB = dbl("qgB", [128, 4, 128], BF16)
        kgB = dbl("kgB", [128, 4, 128], BF16)
        klB = dbl("klB", [128, 512], BF16)
        vbB = dbl("vbB", [128, 1024], BF16)
        sgbB = dbl("sgbB", [128, 1024], F32)
        smaB = dbl("smaB", [128, 1024], F32)
        smbB = dbl("smbB", [128, 1024], F32)
        oaB = dbl("oaB", [128, 4, 128], BF16)
        xB = dbl("xB", [128, D], F32)
        pB = dbl("pB", [128, 256], F32)
        ATm = sbuf(pb2, f"ATm{l}", [128, 4, 128], BF16)
        junk2 = sbuf(pb2, f"junk2{l}", [128, D], BF16)
        ssq4 = sbuf(pb2, f"ssq4{l}", [128, 4], F32)
        rs4 = sbuf(pb2, f"rs4{l}", [128, 4], F32)
        ob = sbuf(pb2, f"ob{l}", [128, D], BF16)
        obT = sbuf(pb2, f"obT{l}", [128, 8, 128], BF16)
        y1 = sbuf(pb2, f"y1{l}", [128, D], F32)
        y2 = sbuf(pb2, f"y2{l}", [128, D], F32)
        ybf = sbuf(pb2, f"ybf{l}", [128, D], BF16)
        yT = sbuf(pb2, f"yT{l}", [128, 8, 128], BF16)
        ssq2 = sbuf(pb2, f"ssq2{l}", [128, 2], F32)
        ssq1 = sbuf(pb2, f"ssq1{l}", [128, 1], F32)
        rs1 = sbuf(pb2, f"rs1{l}", [128, 1], F32)
        t1 = sbuf(pb2, f"t1{l}", [128, D], F32)
        x1 = sbuf(pb2, f"x1{l}", [128, D], F32)
        rbf = sbuf(pb2, f"rbf{l}", [128, D], BF16)
        rT = sbuf(pb2, f"rT{l}", [128, 8, 128], BF16)
        pbf = sbuf(pb2, f"pbf{l}", [128, 256], BF16)
        pT = sbuf(pb2, f"pT{l}", [128, 2, 128], BF16)
        sg = sbuf(pb2, f"sg{l}", [128, D], F32)
        ee = sbuf(pb2, f"ee{l}", [128, D], F32)
        xo = dbl("xo", [128, D], F32)

        def transpose8(src_T, dst_T, n=8):
            for k in range(n):
                P.op("pe", lambda e, k=k: e.transpose(out=psT[:, k * 128:(k + 1) * 128], in_=src_T.t[:, k * 128:(k + 1) * 128], identity=identb.t[:]),
                     reads=[src_T.b, identb.b], writes=[pb[0]])
            P.op("act", lambda e: e.copy(out=dst_T.t[:], in_=psT[:, 0:n * 128].rearrange("p (k t) -> p k t", k=n)), reads=[pb[0]], writes=[dst_T.b])

        def mm_tok(lhs_T, W, nk, banks):
            for hf in range(2):
                for k in range(nk):
                    P.op("pe", lambda e, k=k, hf=hf: e.matmul(out=ps.t[:, banks[hf], :], lhsT=lhs_T.t[:, k, :], rhs=W.t[:, k, hf * 512:(hf + 1) * 512], start=(k == 0), stop=(k == nk - 1)),
                         reads=[lhs_T.b, W.b], writes=[pb[banks[hf]]])

        def sumsq_psum(banks, dst_T):
            for hf in range(2):
                P.op("act", lambda e, hf=hf: e.activation(out=junk2.t[:, hf * 512:(hf + 1) * 512], in_=ps.t[:, banks[hf], :], func=AF.Square, accum_out=ssq2.t[:, hf:hf + 1]),
                     reads=[pb[banks[hf]]], writes=[junk2.b, ssq2.b])
            P.op("dve", lambda e: e.tensor_tensor(out=dst_T.t[:], in0=ssq2.t[:, 0:1], in1=ssq2.t[:, 1:2], op=ALU.add), reads=[ssq2.b], writes=[dst_T.b])

        for b in range(NB):
            i2 = b % 2
            qg_, kg_, kl_, vb_, sgb_, sma_, smb_, oa_, x_, p_ = (qgB[i2], kgB[i2], klB[i2], vbB[i2], sgbB[i2], smaB[i2], smbB[i2], oaB[i2], xB[i2], pB[i2])
            ld(qg_, qg_.t[:], s_qgT[b])
            ld(kg_, kg_.t[:], s_kgT[b])
            ld(kl_, kl_.t[:], s_kl[b])
            ld(vb_, vb_.t[:], s_vb[b])
            ld(sgb_, sgb_.t[:], s_sgb[b])
            ld(sma_, sma_.t[:], s_sma[b])
            ld(smb_, smb_.t[:], s_smb[b])
            ld(oa_, oa_.t[:], s_oagT[b])
            ld(x_, x_.t[:], x_src[b * 128:(b + 1) * 128, :])
            ld(p_, p_.t[:], p_in[l, b * 128:(b + 1) * 128, :])
            for h in range(4):
                P.op("pe", lambda e, h=h: e.matmul(out=ps.t[:, 0, h * 128:(h + 1) * 128], lhsT=kg_.t[:, h, :], rhs=qg_.t[:, h, :], start=True, stop=True),
                     reads=[kg_.b, qg_.b], writes=[pb[0]])
            P.op("dve", lambda e: e.tensor_tensor(out=ATm.t[:], in0=ps.t[:, 0, :].rearrange("p (h t) -> p h t", h=4), in1=trib.t[:].unsqueeze(1).to_broadcast([128, 4, 128]), op=ALU.mult),
                 reads=[pb[0], trib.b], writes=[ATm.b])
            for h in range(4):
                bank = 1 + h // 2
                cs = slice((h % 2) * 256, (h % 2 + 1) * 256)
                P.op("pe", lambda e, h=h, bank=bank, cs=cs: e.matmul(out=ps.t[:, bank, cs], lhsT=ATm.t[:, h, :], rhs=vb_.t[:, h * 256:(h + 1) * 256], start=True, stop=False),
                     reads=[ATm.b, vb_.b], writes=[pb[bank]])
                P.op("pe", lambda e, h=h, bank=bank, cs=cs: e.matmul(out=ps.t[:, bank, cs], lhsT=qg_.t[:, h, :], rhs=S_b.t[:, h, :], start=False, stop=True),
                     reads=[qg_.b, S_b.b], writes=[pb[bank]])
            for h in range(4):
                bank = 3 + h // 2
                cs = slice((h % 2) * 256, (h % 2 + 1) * 256)
                P.op("pe", lambda e, h=h, bank=bank, cs=cs: e.matmul(out=ps.t[:, bank, cs], lhsT=kl_.t[:, h * 128:(h + 1) * 128], rhs=vb_.t[:, h * 256:(h + 1) * 256], start=True, stop=True),
                     reads=[kl_.b, vb_.b], writes=[pb[bank]])
            for h in range(4):
                bank = 3 + h // 2
                cs = slice((h % 2) * 256, (h % 2 + 1) * 256)
                P.op("dve", lambda e, h=h, bank=bank, cs=cs: e.scalar_tensor_tensor(out=S_f.t[:, h, :], in0=S_f.t[:, h, :], scalar=eGl.t[:, b, h:h + 1], in1=ps.t[:, bank, cs], op0=ALU.mult, op1=ALU.add),
                     reads=[S_f.b, eGl.b, pb[bank]], writes=[S_f.b])
            P.op("pool", lambda e: e.tensor_copy(out=S_b.t[:], in_=S_f.t[:]), reads=[S_f.b], writes=[S_b.b])
            for h in range(4):
                bank = 1 + h // 2
                cs = slice((h % 2) * 256, (h % 2 + 1) * 256)
                P.op("act", lambda e, h=h, bank=bank, cs=cs: e.activation(out=junk2.t[:, h * 256:(h + 1) * 256], in_=ps.t[:, bank, cs], func=AF.Square, accum_out=ssq4.t[:, h:h + 1]),
                     reads=[pb[bank]], writes=[junk2.b, ssq4.b])
            rsqrt_col(ssq4, ssq4.t[:], rs4, rs4.t[:], 4, 1.0 / 256, ncols=4)
            for h in range(4):
                bank = 1 + h // 2
                cs = slice((h % 2) * 256, (h % 2 + 1) * 256)
                P.op("dve", lambda e, h=h, bank=bank, cs=cs: e.scalar_tensor_tensor(out=ob.t[:, h * 256:(h + 1) * 256], in0=ps.t[:, bank, cs], scalar=rs4.t[:, h:h + 1], in1=sgb_.t[:, h * 256:(h + 1) * 256], op0=ALU.mult, op1=ALU.mult),
                     reads=[pb[bank], rs4.b, sgb_.b], writes=[ob.b])
            transpose8(ob, obT)
            mm_tok(oa_, Wpa, 4, (1, 2))
            mm_tok(obT, Wpb, 8, (3, 4))
            for hf in range(2):
                cs = slice(hf * 512, (hf + 1) * 512)
                P.op("dve", lambda e, hf=hf, cs=cs: e.tensor_tensor(out=y1.t[:, cs], in0=ps.t[:, 1 + hf, :], in1=sma_.t[:, cs], op=ALU.mult), reads=[pb[1 + hf], sma_.b], writes=[y1.b])
                P.op("dve", lambda e, hf=hf, cs=cs: e.tensor_tensor(out=y2.t[:, cs], in0=ps.t[:, 3 + hf, :], in1=smb_.t[:, cs], op=ALU.mult), reads=[pb[3 + hf], smb_.b], writes=[y2.b])
            P.op("pool", lambda e: e.tensor_tensor(out=ybf.t[:], in0=y1.t[:], in1=y2.t[:], op=ALU.add), reads=[y1.b, y2.b], writes=[ybf.b])
            transpose8(ybf, yT)
            mm_tok(yT, Wo, 8, (5, 6))
            sumsq_psum((5, 6), ssq1)
            rsqrt_col(ssq1, ssq1.t[:], rs1, rs1.t[:], 1, 1.0 / D)
            for hf in range(2):
                cs = slice(hf * 512, (hf + 1) * 512)
                P.op("dve", lambda e, hf=hf, cs=cs: e.scalar_tensor_tensor(out=t1.t[:, cs], in0=ps.t[:, 5 + hf, :], scalar=rs1.t[:], in1=gpost_t.t[:, cs], op0=ALU.mult, op1=ALU.mult),
                     reads=[pb[5 + hf], rs1.b, gpost_t.b], writes=[t1.b])
            P.op("pool", lambda e: e.tensor_tensor(out=x1.t[:], in0=t1.t[:], in1=x_.t[:], op=ALU.add), reads=[t1.b, x_.b], writes=[x1.b])
            P.op("act", lambda e: e.activation(out=junk2.t[:], in_=x1.t[:], func=AF.Square, accum_out=ssq1.t[:]), reads=[x1.b], writes=[junk2.b, ssq1.b])
            rsqrt_col(ssq1, ssq1.t[:], rs1, rs1.t[:], 1, 1.0 / D)
            P.op("dve", lambda e: e.tensor_scalar(out=rbf.t[:], in0=x1.t[:], scalar1=rs1.t[:], scalar2=None, op0=ALU.mult), reads=[x1.b, rs1.b], writes=[rbf.b])
            transpose8(rbf, rT)
            mm_tok(rT, Wpg, 8, (5, 6))
            P.op("act", lambda e: e.copy(out=pbf.t[:], in_=p_.t[:]), reads=[p_.b], writes=[pbf.b])
            transpose8(pbf, pT, n=2)
            mm_tok(pT, Wple, 2, (1, 2))
            for hf in range(2):
                cs = slice(hf * 512, (hf + 1) * 512)
                P.op("act", lambda e, hf=hf, cs=cs: e.activation(out=sg.t[:, cs], in_=ps.t[:, 5 + hf, :], func=AF.Sigmoid), reads=[pb[5 + hf]], writes=[sg.b])
                P.op("dve", lambda e, hf=hf, cs=cs: e.tensor_tensor(out=ee.t[:, cs], in0=ps.t[:, 1 + hf, :], in1=sg.t[:, cs], op=ALU.mult), reads=[pb[1 + hf], sg.b], writes=[ee.b])
            P.op("act", lambda e: e.activation(out=junk2.t[:], in_=ee.t[:], func=AF.Square, accum_out=ssq1.t[:]), reads=[ee.b], writes=[junk2.b, ssq1.b])
            rsqrt_col(ssq1, ssq1.t[:], rs1, rs1.t[:], 1, 1.0 / D)
            P.op("dve", lambda e: e.scalar_tensor_tensor(out=t1.t[:], in0=ee.t[:], scalar=rs1.t[:], in1=gpp_t.t[:], op0=ALU.mult, op1=ALU.mult),
                 reads=[ee.b, rs1.b, gpp_t.b], writes=[t1.b])
            xo_ = xo[i2]
            P.op("pool", lambda e, xo_=xo_: e.tensor_tensor(out=xo_.t[:], in0=t1.t[:], in1=x1.t[:], op=ALU.add), reads=[t1.b, x1.b], writes=[xo_.b])
            P.dma("pool", lambda e, xo_=xo_, b=b: e.dma_start(out=x_dst[b * 128:(b + 1) * 128, :], in_=xo_.t[:]), xo_.b, reads=[xo_.b])
        P.end_phase()
        pb2.close()

    glob.close()
    P.close()
    return nc, P


_CACHE = {}


def _consts(NIT=16):
    j = np.arange(128)[:, None]
    i = np.arange(128)[None, :]
    return {
        "c_ident": np.eye(128, dtype=np.float32),
        "c_tri": (j <= i).astype(np.float32),
        "c_triu": (j > i).astype(np.float32),
        "c_cmask": np.where(i <= j, 0.0, NEG).astype(np.float32),
        "c_pw": np.broadcast_to((0.5 ** (np.arange(NIT) + 1)).astype(np.float32), (128, NIT)).copy(),
    }


def make_in_map(xb, pb_, wts):
    m = {"x": np.ascontiguousarray(xb), "p": np.ascontiguousarray(pb_)}
    m.update(wts)
    return m


def prep_weights(g_pre, w_in, w_gate_up, b_gate, g_gla_head, w_proj_a, w_proj_b, w_out, g_post, w_ple,
                 w_ple_gate, g_ple_pre, g_ple_post):
    dep = g_pre.shape[0]
    c = lambda a: np.ascontiguousarray(np.asarray(a, dtype=np.float32))
    w = {
        "w_in": c(w_in), "gpreT": c(np.asarray(g_pre).reshape(dep, 8, 128).transpose(0, 2, 1)),
        "w_gate_up": c(w_gate_up), "b_gate": c(b_gate),
        "gglaT": c(np.asarray(g_gla_head).reshape(dep, 2, 128).transpose(0, 2, 1)),
        "w_proj_a": c(w_proj_a), "w_proj_b": c(w_proj_b), "w_out": c(w_out), "g_post": c(g_post),
        "w_ple": c(w_ple), "w_ple_gate": c(w_ple_gate),
        "gppT": c(np.asarray(g_ple_pre).reshape(dep, 8, 128).transpose(0, 2, 1)), "g_ple_post": c(g_ple_post),
    }
    w.update(_consts())
    return w


def kernel(x, p, g_pre, w_in, w_gate_up, b_gate, g_gla_head, w_proj_a, w_proj_b, w_out, g_post, w_ple,
           w_ple_gate, g_ple_pre, g_ple_post):
    x = np.asarray(x, dtype=np.float32)
    p = np.asarray(p, dtype=np.float32)
    B, L, _ = x.shape
    if L not in _CACHE:
        _CACHE[L] = build(L)[0]
    nc = _CACHE[L]
    wts = prep_weights(g_pre, w_in, w_gate_up, b_gate, g_gla_head, w_proj_a, w_proj_b, w_out, g_post, w_ple,
                       w_ple_gate, g_ple_pre, g_ple_post)
    in_maps = [make_in_map(x[b], p[:, b], wts) for b in range(B)]
    res = run_bass_kernel_spmd(nc, in_maps, core_ids=list(range(B)))
    return np.stack([np.asarray(r["y"], dtype=np.float32) for r in res.results], axis=0)
```
